# Optimizing a Trainium2 kernel written in Bass

```python
import math
import jax
import jax.numpy as jnp
from jax import lax
import numpy as np

D_MODEL = 1024
BATCH = 8
SEQ = 8192
DEPTH = 1

EPS = 1e-6
NEG_INF = -1e30
DN_HEADS = 4
DN_DK = 128
DN_DV = 128
DN_KEY_W = DN_HEADS * DN_DK
DN_VAL_W = DN_HEADS * DN_DV
DN_CONV = 4
DN_CHUNK = 64
N_DIR = 2
DA_GROUPS = ((128, 1), (512, 4), (2048, 16))
DA_HEADS = 4
DA_HD = 128
DA_W = len(DA_GROUPS) * DA_HEADS * DA_HD
RP_BUCKETS = 32
RP_MAX_DIST = 1024
PEER_HEADS = 8
PEER_NKEYS = 128
PEER_QDIM = 256
PEER_TOPK = 16
PEER_EXPERTS = PEER_NKEYS * PEER_NKEYS
PEER_TOKEN_BLOCK = 128

IN_WIDTHS = (DN_KEY_W, DN_KEY_W, DN_VAL_W, DN_VAL_W, N_DIR * DN_HEADS, N_DIR * DN_HEADS,
             DA_W, DA_W, DA_W, D_MODEL, D_MODEL)

kernel_name = 'hybrid_deltanet_dilated_peer_encoder'


def rmsnorm(x, g):
    xf = x.astype(jnp.float32)
    y = xf * lax.rsqrt(jnp.mean(xf * xf, axis=-1, keepdims=True) + EPS)
    return (y * g.astype(jnp.float32)).astype(x.dtype)


def l2norm(x):
    return x * lax.rsqrt(jnp.sum(x * x, axis=-1, keepdims=True) + EPS)


def split_columns(t, widths):
    out, start = [], 0
    for w in widths:
        out.append(t[..., start:start + w])
        start += w
    return out


def centred_depthwise_conv(x, w):
    k, c = w.shape
    left = k // 2
    return lax.conv_general_dilated(x, w[:, None, :], window_strides=(1,), padding=[(left, k - 1 - left)],
                                    dimension_numbers=('NWC', 'WIO', 'NWC'), feature_group_count=c)


def gated_delta_rule(q, k, v, g, beta):
    r, s, dk = q.shape
    dv = v.shape[-1]
    c = DN_CHUNK
    n = s // c
    q = (q * (dk ** -0.5)).reshape(r, n, c, dk)
    k = k.reshape(r, n, c, dk)
    v = v.reshape(r, n, c, dv)
    beta = beta.reshape(r, n, c)
    gcum = jnp.cumsum(g.reshape(r, n, c), axis=-1)
    diff = gcum[..., :, None] - gcum[..., None, :]
    lower = jnp.tril(jnp.ones((c, c), dtype=bool))
    strict = jnp.tril(jnp.ones((c, c), dtype=bool), -1)
    decay = jnp.where(lower, jnp.exp(jnp.where(lower, diff, 0.0)), 0.0)
    kb = k * beta[..., None]
    lmat = jnp.where(strict, jnp.einsum('rnid,rnjd->rnij', kb, k) * decay, 0.0)
    amat = jnp.eye(c, dtype=jnp.float32) + lmat
    rhs = jnp.concatenate([v * beta[..., None], kb * jnp.exp(gcum)[..., None]], axis=-1)
    sol = lax.linalg.triangular_solve(amat, rhs, left_side=True, lower=True, unit_diagonal=True)
    u, w = sol[..., :dv], sol[..., dv:]
    intra = jnp.einsum('rnid,rnjd->rnij', q, k) * decay
    q_dec = q * jnp.exp(gcum)[..., None]
    k_dec = k * jnp.exp(gcum[..., -1:] - gcum)[..., None]
    g_last = jnp.exp(gcum[..., -1])

    def step(state, inp):
        u_i, w_i, intra_i, qd_i, kd_i, gl_i = inp
        v_new = u_i - jnp.einsum('rcd,rde->rce', w_i, state)
        out = jnp.einsum('rcd,rde->rce', qd_i, state) + jnp.einsum('rij,rje->rie', intra_i, v_new)
        state = state * gl_i[:, None, None] + jnp.einsum('rcd,rce->rde', kd_i, v_new)
        return state, out

    xs = tuple(jnp.moveaxis(a, 1, 0) for a in (u, w, intra, q_dec, k_dec, g_last))
    _, out = lax.scan(step, jnp.zeros((r, dk, dv), jnp.float32), xs)
    return jnp.moveaxis(out, 0, 1).reshape(r, s, dv)


def deltanet_branch(q, k, v, z, a, b, conv_w, a_log, dt_bias, out_norm):
    bsz, s, _ = q.shape
    f32 = jnp.float32
    qkv = jax.nn.silu(centred_depthwise_conv(jnp.concatenate([q, k, v], axis=-1), conv_w))
    q, k, v = split_columns(qkv, (DN_KEY_W, DN_KEY_W, DN_VAL_W))

    def heads(t, d):
        return t.reshape(bsz, s, DN_HEADS, d).transpose(0, 2, 1, 3).astype(f32)

    def per_dir(t):
        return t.reshape(bsz, s, N_DIR, DN_HEADS).transpose(2, 0, 3, 1).astype(f32)

    q, k, v = l2norm(heads(q, DN_DK)), l2norm(heads(k, DN_DK)), heads(v, DN_DV)
    g = -jnp.exp(a_log.astype(f32))[:, None, :, None] * jax.nn.softplus(per_dir(a) + dt_bias.astype(f32)[:, None, :, None])
    beta = jax.nn.sigmoid(per_dir(b))
    qs = jnp.stack([q, jnp.flip(q, 2)])
    ks = jnp.stack([k, jnp.flip(k, 2)])
    vs = jnp.stack([v, jnp.flip(v, 2)])
    gs = jnp.stack([g[0], jnp.flip(g[1], -1)])
    bs = jnp.stack([beta[0], jnp.flip(beta[1], -1)])
    rows = N_DIR * bsz * DN_HEADS
    o = gated_delta_rule(qs.reshape(rows, s, DN_DK), ks.reshape(rows, s, DN_DK), vs.reshape(rows, s, DN_DV),
                         gs.reshape(rows, s), bs.reshape(rows, s)).reshape(N_DIR, bsz, DN_HEADS, s, DN_DV)
    o = (o[0] + jnp.flip(o[1], 2)).transpose(0, 2, 1, 3)
    o = rmsnorm(o, out_norm) * jax.nn.silu(z.reshape(bsz, s, DN_HEADS, DN_DV).astype(f32))
    return o.reshape(bsz, s, DN_VAL_W)


def t5_bucket(rel):
    half = RP_BUCKETS // 2
    max_exact = half // 2
    n = np.abs(rel)
    large = max_exact + (np.log(np.maximum(n, 1) / max_exact) / math.log(RP_MAX_DIST / max_exact)
                         * (half - max_exact)).astype(np.int64)
    large = np.minimum(large, half - 1)
    return np.where(rel > 0, half, 0) + np.where(n < max_exact, n, large)


def dilated_group_attention(q, k, v, bias_table, window, dil):
    bsz, h, s, hd = q.shape
    side = window // (2 * dil)
    length = s // dil
    nb = -(-length // side)
    padded = nb * side

    def residue_blocks(t):
        t = t.reshape(bsz, h, length, dil, hd).transpose(0, 1, 3, 2, 4)
        t = jnp.pad(t, ((0, 0), (0, 0), (0, 0), (0, padded - length), (0, 0)))
        return t.reshape(bsz, h, dil, nb, side, hd)

    def with_neighbours(t):
        tp = jnp.pad(t, ((0, 0), (0, 0), (0, 0), (1, 1), (0, 0), (0, 0)))
        return jnp.concatenate([tp[:, :, :, :-2], tp[:, :, :, 1:-1], tp[:, :, :, 2:]], axis=4)

    qb = residue_blocks(q)
    kn = with_neighbours(residue_blocks(k))
    vn = with_neighbours(residue_blocks(v)).astype(jnp.float32)
    rel = np.arange(3 * side)[None, :] - side - np.arange(side)[:, None]
    key_idx = np.arange(nb)[:, None, None] * side + np.arange(3 * side)[None, None, :] - side
    valid = (np.abs(rel) <= side)[None] & (key_idx >= 0) & (key_idx < length)
    bias = bias_table.astype(jnp.float32)[:, t5_bucket(rel * dil)]
    scores = jnp.einsum('bhrnqd,bhrnkd->bhrnqk', qb, kn, preferred_element_type=jnp.float32) * (hd ** -0.5)
    scores = jnp.where(valid, scores + bias[None, :, None, None], NEG_INF)
    m = jnp.max(scores, axis=-1, keepdims=True)
    p = jnp.exp(scores - m)
    l = jnp.sum(p, axis=-1)
    o = jnp.einsum('bhrnqk,bhrnkd->bhrnqd', p, vn) / l[..., None]
    lse = m[..., 0] + jnp.log(l)
    o = o.reshape(bsz, h, dil, padded, hd)[:, :, :, :length].transpose(0, 1, 3, 2, 4).reshape(bsz, h, s, hd)
    lse = lse.reshape(bsz, h, dil, padded)[..., :length].transpose(0, 1, 3, 2).reshape(bsz, h, s)
    return o, lse


def dilated_attention_branch(q, k, v, rel_bias):
    bsz, s, _ = q.shape

    def groups(t):
        return t.reshape(bsz, s, len(DA_GROUPS), DA_HEADS, DA_HD).transpose(2, 0, 3, 1, 4)

    qg, kg, vg = groups(q), groups(k), groups(v)
    outs, lses = [], []
    for gi, (window, dil) in enumerate(DA_GROUPS):
        table = rel_bias[:, gi * DA_HEADS:(gi + 1) * DA_HEADS].T
        o, lse = dilated_group_attention(qg[gi], kg[gi], vg[gi], table, window, dil)
        outs.append(o)
        lses.append(lse)
    wts = jax.nn.softmax(jnp.stack(lses), axis=0)
    o = jnp.einsum('gbhs,gbhsd->bhsd', wts, jnp.stack(outs))
    return o.transpose(0, 2, 1, 3).reshape(bsz, s, DA_HEADS * DA_HD)


def peer_ffn(xn, w_q, sub_keys, expert_u, expert_v):
    bsz, s, d = xn.shape
    t = bsz * s
    x2 = xn.reshape(t, d)
    qh = (x2 @ w_q).reshape(t, PEER_HEADS, 2, PEER_QDIM // 2)
    scores = jnp.einsum('thcd,hckd->thck', qh, sub_keys, preferred_element_type=jnp.float32)
    sv, si = lax.top_k(scores, PEER_TOPK)
    cand = sv[..., 0, :, None] + sv[..., 1, None, :]
    cv, ci = lax.top_k(cand.reshape(t, PEER_HEADS, PEER_TOPK * PEER_TOPK), PEER_TOPK)
    i1 = jnp.take_along_axis(si[..., 0, :], ci // PEER_TOPK, axis=-1)
    i2 = jnp.take_along_axis(si[..., 1, :], ci % PEER_TOPK, axis=-1)
    experts = (i1 * PEER_NKEYS + i2).reshape(t, PEER_HEADS * PEER_TOPK)
    gates = jax.nn.softmax(cv, axis=-1).reshape(t, PEER_HEADS * PEER_TOPK)

    def token_block(args):
        xb, eb, gb = args
        act = jax.nn.gelu(jnp.einsum('tkd,td->tk', expert_u[eb], xb, preferred_element_type=jnp.float32))
        return jnp.einsum('tk,tkd->td', act * gb, expert_v[eb].astype(jnp.float32)).astype(xb.dtype)

    nblk = t // PEER_TOKEN_BLOCK
    out = lax.map(token_block, (x2.reshape(nblk, PEER_TOKEN_BLOCK, d),
                                experts.reshape(nblk, PEER_TOKEN_BLOCK, -1),
                                gates.reshape(nblk, PEER_TOKEN_BLOCK, -1)))
    return out.reshape(bsz, s, d)


def setup_inputs(seed: int = 0) -> dict:
    key = jax.random.key(seed)
    ks = jax.random.split(key, 18)
    f32 = jnp.float32

    def normal(k, shape, scale):
        return jax.random.normal(k, shape, f32) * scale

    dt = jnp.exp(jax.random.uniform(ks[5], (DEPTH, N_DIR, DN_HEADS), f32, math.log(1e-3), math.log(1e-1)))
    return {
        'x': normal(ks[0], (BATCH, SEQ, D_MODEL), 1.0),
        'norm_mix': 1.0 + normal(ks[1], (DEPTH, D_MODEL), 0.02),
        'w_in': normal(ks[2], (DEPTH, D_MODEL, sum(IN_WIDTHS)), D_MODEL ** -0.5),
        'dn_conv': normal(ks[3], (DEPTH, DN_CONV, 2 * DN_KEY_W + DN_VAL_W), DN_CONV ** -0.5),
        'dn_a_log': jnp.log(jax.random.uniform(ks[4], (DEPTH, N_DIR, DN_HEADS), f32, 1.0, 16.0)),
        'dn_dt_bias': dt + jnp.log(-jnp.expm1(-dt)),
        'dn_out_norm': 1.0 + normal(ks[6], (DEPTH, DN_DV), 0.02),
        'rel_bias': normal(ks[7], (RP_BUCKETS, len(DA_GROUPS) * DA_HEADS), 0.2),
        'w_branch_dn': normal(ks[8], (DEPTH, DN_VAL_W, D_MODEL), DN_VAL_W ** -0.5),
        'w_branch_da': normal(ks[9], (DEPTH, DA_HEADS * DA_HD, D_MODEL), (DA_HEADS * DA_HD) ** -0.5),
        'w_out': normal(ks[10], (DEPTH, D_MODEL, D_MODEL), D_MODEL ** -0.5),
        'norm_ffn': 1.0 + normal(ks[11], (DEPTH, D_MODEL), 0.02),
        'peer_wq': normal(ks[12], (DEPTH, D_MODEL, PEER_HEADS * PEER_QDIM), D_MODEL ** -0.5),
        'peer_sub_keys': normal(ks[13], (DEPTH, PEER_HEADS, 2, PEER_NKEYS, PEER_QDIM // 2), (PEER_QDIM // 2) ** -0.5),
        'peer_u': normal(ks[14], (DEPTH, PEER_EXPERTS, D_MODEL), D_MODEL ** -0.5),
        'peer_v': normal(ks[15], (DEPTH, PEER_EXPERTS, D_MODEL), PEER_HEADS ** -0.5),
        'norm_final': 1.0 + normal(ks[16], (D_MODEL,), 0.02),
    }


def reference(x, norm_mix, w_in, dn_conv, dn_a_log, dn_dt_bias, dn_out_norm, rel_bias, w_branch_dn,
              w_branch_da, w_out, norm_ffn, peer_wq, peer_sub_keys, peer_u, peer_v, norm_final):
    h = x
    for layer in range(DEPTH):
        xn = rmsnorm(h, norm_mix[layer])
        (dq, dk, dv, dz, da, db, aq, ak, av, gate_dn, gate_da) = split_columns(xn @ w_in[layer], IN_WIDTHS)
        o_dn = deltanet_branch(dq, dk, dv, dz, da, db, dn_conv[layer], dn_a_log[layer], dn_dt_bias[layer],
                               dn_out_norm[layer]).astype(h.dtype)
        o_da = dilated_attention_branch(aq, ak, av, rel_bias).astype(h.dtype)
        merged = (jax.nn.sigmoid(gate_dn) * (o_dn @ w_branch_dn[layer])
                  + jax.nn.sigmoid(gate_da) * (o_da @ w_branch_da[layer]))
        h = h + merged @ w_out[layer]
        h = h + peer_ffn(rmsnorm(h, norm_ffn[layer]), peer_wq[layer], peer_sub_keys[layer],
                         peer_u[layer], peer_v[layer])
    return rmsnorm(h, norm_final)
```

```python
import math
from contextlib import ExitStack

import numpy as np
import concourse.bass as bass
import concourse.mybir as mybir
from concourse.bass_utils import run_bass_kernel_spmd

F32 = mybir.dt.float32
BF16 = mybir.dt.bfloat16
I32 = mybir.dt.int32
U32 = mybir.dt.uint32
AF = mybir.ActivationFunctionType
ALU = mybir.AluOpType
AX = mybir.AxisListType

P = 128
D = 1024
KC = D // P
NW = 8720
EPS = 1e-6
SEM_LIMIT = 24000


class KB:
    def __init__(self, nc, es):
        self.nc, self.es = nc, es
        self.E = {'pe': nc.tensor, 'act': nc.scalar, 'dve': nc.vector, 'pool': nc.gpsimd, 'sp': nc.sync}
        self.esem = {}
        self.seen = {e: {} for e in self.E}
        self.W = {}
        self.R = {}
        self.dsem = {}
        self.nsem = 0
        self.ninst = {e: 0 for e in self.E}
        self.excl = set()
        self.dma_sids = set()

    def newsem(self, name):
        self.nsem += 1
        return self.es.enter_context(self.nc.semaphore(f"{name}_{self.nsem}"))

    def _esem(self, eng):
        s = self.esem.get(eng)
        if s is None or s[1] >= SEM_LIMIT:
            s = [self.newsem("e" + eng), 0]
            self.esem[eng] = s
        return s

    @staticmethod
    def _merge(deps, d):
        for sid, (sem, val) in d.items():
            if sid not in deps or deps[sid][1] < val:
                deps[sid] = (sem, val)

    def _wait(self, eng, deps):
        for sid, (sem, val) in deps.items():
            if self.seen[eng].get(sid, 0) < val:
                self.E[eng].wait_ge(sem, val)
                self.seen[eng][sid] = val

    def _deps(self, r, w, skip_sid=None):
        deps = {}
        for k in r:
            self._merge(deps, self.W.get(k, {}))
        for k in w:
            self._merge(deps, self.W.get(k, {}))
            self._merge(deps, self.R.get(k, {}))
        if skip_sid is not None:
            deps.pop(skip_sid, None)
        return deps

    def op(self, eng, fn, r=(), w=()):
        if self.excl:
            w = tuple(w) + tuple(k for k in r if k in self.excl and k not in w)
        deps = self._deps(r, w)
        if eng == 'pe':
            s0 = self.esem.get('pe')
            for sid in list(deps):
                if deps[sid][0] is (s0[0] if s0 else None):
                    deps.pop(sid)
        self._wait(eng, deps)
        inst = fn(self.E[eng])
        s = self._esem(eng)
        s[1] += 1
        inst.then_inc(s[0], 1)
        self.ninst[eng] += 1
        sid = id(s[0])
        for k in r:
            self.R.setdefault(k, {})[sid] = (s[0], s[1])
        for k in w:
            self.W[k] = {sid: (s[0], s[1])}
            self.R[k] = {}
        return inst

    def dma(self, q, out, in_, r=(), w=(), acc=False, indirect=None, sk=None, **kw):
        if sk is None:
            sk = ('st', r[0]) if (str(out.space).endswith('DRAM') and r) else w[0]
        ds = self.dsem.get(sk)
        if ds is None or ds[1] >= SEM_LIMIT:
            ds = [self.newsem("d"), 0]
            self.dsem[sk] = ds
            self.dma_sids.add(id(ds[0]))
        sid = id(ds[0])
        deps = {}
        for k in r:
            self._merge(deps, self.W.get(k, {}))
        for k in w:
            if not acc:
                self._merge(deps, self.W.get(k, {}))
            else:
                self._merge(deps, {i: v for i, v in self.W.get(k, {}).items() if i not in self.dma_sids})
            self._merge(deps, self.R.get(k, {}))
        self._wait(q, deps)
        if indirect is None:
            inst = self.E[q].dma_start(out=out, in_=in_, **kw)
        else:
            inst = self.E[q].indirect_dma_start(out=out, out_offset=None, in_=in_, in_offset=indirect)
        ds[1] += 16
        inst.then_inc(ds[0], 16)
        self.ninst[q] += 1
        for k in r:
            self.R.setdefault(k, {})[sid] = (ds[0], ds[1])
        for k in w:
            if acc:
                self.W.setdefault(k, {})[sid] = (ds[0], ds[1])
            else:
                self.W[k] = {sid: (ds[0], ds[1])}
                self.R[k] = {}
        return inst

    def barrier(self):
        deps = {}
        for e, s in self.esem.items():
            if s[1] > 0:
                deps[id(s[0])] = (s[0], s[1])
        for k, s in self.dsem.items():
            if s[1] > 0:
                deps[id(s[0])] = (s[0], s[1])
        for e in self.E:
            self._wait(e, deps)

    def finish(self, keys, eng='sp'):
        deps = self._deps(keys, ())
        self._wait(eng, deps)


_UID = [0]


def sb(nc, es, name, shape, dt):
    _UID[0] += 1
    return es.enter_context(nc.sbuf_tensor(f"{name}_{_UID[0]}", list(shape), dt))


def ps(nc, es, name, shape, dt):
    _UID[0] += 1
    return es.enter_context(nc.psum_tensor(f"{name}_{_UID[0]}", list(shape), dt))


def col_tiles():
    tiles = []
    c = 0
    for nm, wdt, fn, sc in (("dq", 512, None, 1.0), ("dk", 512, None, 1.0), ("dv", 512, None, 1.0),
                            ("dz", 512, AF.Silu, 1.0)):
        for i in range(wdt // P):
            tiles.append((nm, c, P, fn, sc))
            c += P
    tiles.append(("ab", c, 16, None, 1.0))
    c += 16
    for nm, wdt, fn, sc in (("aq", 1536, None, 128 ** -0.5), ("ak", 1536, None, 1.0), ("av", 1536, None, 1.0),
                            ("gdn", 1024, AF.Sigmoid, 1.0), ("gda", 1024, AF.Sigmoid, 1.0)):
        for i in range(wdt // P):
            tiles.append((nm, c, P, fn, sc))
            c += P
    assert c == NW
    return tiles


def phase_a(nc, kb, S, x, w_in, norm_mix, projT, abT):
    tiles = col_tiles()
    ST = 512
    with ExitStack() as es:
        w_bf = sb(nc, es, "w_bf", [P, KC, NW], BF16)
        gmix = sb(nc, es, "gmix", [P, D], F32)
        ident = sb(nc, es, "ident", [P, P], BF16)
        identf = sb(nc, es, "identf", [P, P], F32)
        xt = [sb(nc, es, f"xt{i}", [P, D], F32) for i in range(2)]
        junk = sb(nc, es, "junk", [P, D], BF16)
        ss = [sb(nc, es, f"ss{i}", [P, 2], F32) for i in range(2)]
        xn = [sb(nc, es, f"xn{i}", [P, D], BF16) for i in range(2)]
        xnT = [sb(nc, es, f"xnT{i}", [P, KC, ST], BF16) for i in range(2)]
        ot = [sb(nc, es, f"ot{i}", [P, ST], BF16) for i in range(4)]
        otf = sb(nc, es, "otf", [16, ST], F32)
        pT = [ps(nc, es, f"pT{i}", [P, KC, P], BF16) for i in range(2)]
        pM = [ps(nc, es, f"pM{i}", [P, ST], F32) for i in range(4)]

        kb.dma('sp', gmix[:], norm_mix[0:1, :].partition_broadcast(P), r=('norm_mix',), w=('gmix',))
        kb.op('pool', lambda e: e.memset(identf[:], 0.0), w=('identf',))
        kb.op('pool', lambda e: e.affine_select(out=identf[:], in_=identf[:], pattern=[[-1, P]],
                                                compare_op=ALU.not_equal, fill=1.0, base=0, channel_multiplier=1),
              r=('identf',), w=('identf',))
        kb.op('dve', lambda e: e.tensor_copy(out=ident[:], in_=identf[:]), r=('identf',), w=('ident',))

        NPC = 10
        pw = NW // NPC
        assert pw <= D
        n = 0
        for kc in range(KC):
            for pc in range(NPC):
                s = n % 2
                kb.dma('sp', xt[s][:, 0:pw], w_in[kc * P:(kc + 1) * P, pc * pw:(pc + 1) * pw],
                       r=('w_in',), w=(('xt', s),))
                eng = 'act' if n % 2 == 0 else 'dve'
                if eng == 'act':
                    kb.op('act', lambda e: e.activation(out=w_bf[:, kc, pc * pw:(pc + 1) * pw], in_=xt[s][:, 0:pw],
                                                        func=AF.Copy), r=(('xt', s),), w=('w_bf',))
                else:
                    kb.op('dve', lambda e: e.tensor_copy(out=w_bf[:, kc, pc * pw:(pc + 1) * pw], in_=xt[s][:, 0:pw]),
                          r=(('xt', s),), w=('w_bf',))
                n += 1

        nst = S // ST
        ti = 0
        oi = 0
        pi = 0
        for st in range(nst):
            xs = st % 2
            for j in range(ST // P):
                s = ti % 2
                t0 = st * ST + j * P
                kb.dma('sp', xt[s][:], x[t0:t0 + P, :], r=('x',), w=(('xt', s),))
                kb.op('act', lambda e: e.activation(out=junk[:], in_=xt[s][:], func=AF.Square, scale=D ** -0.5,
                                                    accum_out=ss[s][:, 0:1]),
                      r=(('xt', s),), w=('junk', ('ss', s)))
                kb.op('dve', lambda e: e.tensor_scalar(out=ss[s][:, 1:2], in0=ss[s][:, 0:1], scalar1=EPS,
                                                       scalar2=None, op0=ALU.add),
                      r=(('ss', s),), w=(('ss', s),))
                kb.op('act', lambda e: e.activation(out=ss[s][:, 1:2], in_=ss[s][:, 1:2], func=AF.Sqrt),
                      r=(('ss', s),), w=(('ss', s),))
                kb.op('dve', lambda e: e.reciprocal(out=ss[s][:, 1:2], in_=ss[s][:, 1:2]),
                      r=(('ss', s),), w=(('ss', s),))
                kb.op('dve', lambda e: e.scalar_tensor_tensor(out=xn[s][:], in0=xt[s][:], scalar=ss[s][:, 1:2],
                                                              in1=gmix[:], op0=ALU.mult, op1=ALU.mult),
                      r=(('xt', s), ('ss', s), 'gmix'), w=(('xn', s),))
                for kc in range(KC):
                    kb.op('pe', lambda e: e.transpose(out=pT[s][:, kc, :], in_=xn[s][:, kc * P:(kc + 1) * P],
                                                      identity=ident[:]),
                          r=(('xn', s), 'ident'), w=(('pT', s),))
                kb.op('act', lambda e: e.activation(out=xnT[xs][:, :, j * P:(j + 1) * P], in_=pT[s][:],
                                                    func=AF.Copy),
                      r=(('pT', s),), w=(('xnT', xs),))
                ti += 1
            for (nm, c0, cw, fn, sc) in tiles:
                pb = pi % 4
                pi += 1
                for kc in range(KC):
                    kb.op('pe', lambda e: e.matmul(pM[pb][0:cw, :], lhsT=w_bf[:, kc, c0:c0 + cw],
                                                   rhs=xnT[xs][:, kc, :], start=(kc == 0), stop=(kc == KC - 1)),
                          r=('w_bf', ('xnT', xs)), w=(('pM', pb),))
                if nm == "ab":
                    kb.op('dve', lambda e: e.tensor_copy(out=otf[:], in_=pM[pb][0:16, :]),
                          r=(('pM', pb),), w=('otf',))
                    kb.dma('pool', abT[:, st * ST:(st + 1) * ST], otf[:], r=('otf',), w=('abT',), acc=True)
                    continue
                o = oi % 4
                oi += 1
                if fn is not None:
                    kb.op('act', lambda e: e.activation(out=ot[o][:], in_=pM[pb][:], func=fn),
                          r=(('pM', pb),), w=(('ot', o),))
                elif oi % 2 == 0:
                    kb.op('act', lambda e: e.activation(out=ot[o][:], in_=pM[pb][:], func=AF.Copy, scale=sc),
                          r=(('pM', pb),), w=(('ot', o),))
                else:
                    kb.op('dve', lambda e: e.tensor_scalar(out=ot[o][:], in0=pM[pb][:], scalar1=sc, scalar2=None,
                                                           op0=ALU.mult),
                          r=(('pM', pb),), w=(('ot', o),))
                ct = (c0 if c0 < 2048 else c0 - 16) // P
                kb.dma('pool', projT[ct, :, st * ST:(st + 1) * ST], ot[o][:], r=(('ot', o),), w=('projT',), acc=True)


DA_GROUPS = ((128, 1), (512, 4), (2048, 16))


def t5_bucket_np(rel):
    half = 16
    max_exact = 8
    n = np.abs(rel)
    large = max_exact + (np.log(np.maximum(n, 1) / max_exact) / math.log(1024 / max_exact)
                         * (half - max_exact)).astype(np.int64)
    large = np.minimum(large, half - 1)
    return np.where(rel > 0, half, 0) + np.where(n < max_exact, n, large)


def make_ohv():
    out = np.zeros((33, 6, 256), np.float32)
    for g, (window, dil) in enumerate(DA_GROUPS):
        for ty in range(2):
            for j in range(256):
                d = 127 - j
                rel = d - 64 if ty == 0 else d + 64
                if j < 255 and abs(rel) <= 64:
                    b = int(t5_bucket_np(np.array([rel * dil]))[0])
                    out[b, g * 2 + ty, j] = 1.0
                else:
                    out[32, g * 2 + ty, j] = -1e30
    return out


def make_identities(nc, kb, es):
    identf = sb(nc, es, "identf", [P, P], F32)
    ident = sb(nc, es, "ident", [P, P], BF16)
    kb.op('pool', lambda e: e.memset(identf[:], 0.0), w=('identf',))
    kb.op('pool', lambda e: e.affine_select(out=identf[:], in_=identf[:], pattern=[[-1, P]],
                                            compare_op=ALU.not_equal, fill=1.0, base=0, channel_multiplier=1),
          r=('identf',), w=('identf',))
    kb.op('dve', lambda e: e.tensor_copy(out=ident[:], in_=identf[:]), r=('identf',), w=('ident',))
    return identf, ident


def phase_c(nc, kb, S, projT, rel_bias, c_ohv, hv, odaT):
    PADMAX = 64 * 16
    with ExitStack() as es:
        identf, ident = make_identities(nc, kb, es)
        Jf = sb(nc, es, "Jf", [P, P], F32)
        onesb = sb(nc, es, "onesb", [P, P], BF16)
        tblx = sb(nc, es, "tblx", [33, 12], F32)
        ohv = sb(nc, es, "ohv", [33, 6, 256], F32)
        hvs = sb(nc, es, "hvs", [4, 256], F32)
        hk = [sb(nc, es, f"hk{i}", [P, P], F32) for i in range(2)]
        bt = sb(nc, es, "bt", [P, 12, 4, P], BF16)
        ebt = sb(nc, es, "ebt", [P, 12, 4, P], BF16)
        QT = sb(nc, es, "QT", [P, S], BF16)
        KTp = sb(nc, es, "KTp", [P, S + 2 * PADMAX], BF16)
        VTp = sb(nc, es, "VTp", [P, S + 2 * PADMAX], BF16)
        Vtok = sb(nc, es, "Vtok", [P, 80, P], BF16)
        numacc = sb(nc, es, "numacc", [P, S], F32)
        denacc = sb(nc, es, "denacc", [P, S], F32)
        pt = [sb(nc, es, f"pt{i}", [P, P], BF16) for i in range(4)]
        pS = [ps(nc, es, f"pS{i}", [P, P], F32) for i in range(2)]
        pN = [ps(nc, es, f"pN{i}", [P, P], F32) for i in range(2)]
        pD = [ps(nc, es, f"pD{i}", [P, P], F32) for i in range(2)]
        pV = [ps(nc, es, f"pV{i}", [P, P], BF16) for i in range(2)]

        kb.op('pool', lambda e: e.memset(Jf[:], 0.0), w=('Jf',))
        kb.op('pool', lambda e: e.affine_select(out=Jf[:], in_=Jf[:], pattern=[[1, P]],
                                                compare_op=ALU.not_equal, fill=1.0, base=-(P - 1),
                                                channel_multiplier=1), r=('Jf',), w=('Jf',))
        kb.op('pool', lambda e: e.memset(onesb[:], 1.0), w=('onesb',))
        kb.op('pool', lambda e: e.memset(tblx[:], 1.0), w=('tblx',))
        kb.dma('sp', tblx[0:32, :], rel_bias[:, :], r=('rel_bias',), w=('tblx',))
        kb.dma('sp', ohv[:], c_ohv[:, :, :], r=('c_ohv',), w=('ohv',))
        n = 0
        for g in range(3):
            for ty in range(2):
                kb.op('pe', lambda e: e.matmul(pN[0][0:4, :], lhsT=tblx[:, g * 4:(g + 1) * 4],
                                               rhs=ohv[:, g * 2 + ty, 0:P], start=True, stop=True),
                      r=('tblx', 'ohv'), w=(('pN', 0),))
                kb.op('pe', lambda e: e.matmul(pN[1][0:4, :], lhsT=tblx[:, g * 4:(g + 1) * 4],
                                               rhs=ohv[:, g * 2 + ty, P:2 * P], start=True, stop=True),
                      r=('tblx', 'ohv'), w=(('pN', 1),))
                kb.op('dve', lambda e: e.tensor_copy(out=hvs[:, 0:P], in_=pN[0][0:4, :]), r=(('pN', 0),), w=('hvs',))
                kb.op('dve', lambda e: e.tensor_copy(out=hvs[:, P:2 * P], in_=pN[1][0:4, :]), r=(('pN', 1),),
                      w=('hvs',))
                kb.dma('sp', hv[(g * 2 + ty) * 4:(g * 2 + ty) * 4 + 4, :], hvs[:], r=('hvs',), w=('hv',))
                for h in range(4):
                    s = n % 2
                    n += 1
                    src = bass.AP(tensor=hv.tensor, offset=((g * 2 + ty) * 4 + h) * 256, ap=[[1, P], [1, P]])
                    kb.dma('sp', hk[s][:], src, r=('hv',), w=(('hk', s),))
                    kb.op('pe', lambda e: e.matmul(pS[s][:], lhsT=Jf[:], rhs=hk[s][:], start=True, stop=True),
                          r=('Jf', ('hk', s)), w=(('pS', s),))
                    gh = g * 4 + h
                    kb.op('dve', lambda e: e.tensor_copy(out=bt[:, gh, ty, :], in_=pS[s][:]), r=(('pS', s),),
                          w=('bt',))
                    kb.op('dve', lambda e: e.tensor_copy(out=bt[:, gh, 2 + ty, :], in_=pS[s][:]), r=(('pS', s),),
                          w=('bt',))
                    if ty == 0:
                        kb.op('pool', lambda e: e.memset(bt[0:64, gh, 2, :], -1e30), r=('bt',), w=('bt',))
                    else:
                        kb.op('pool', lambda e: e.memset(bt[64:128, gh, 3, :], -1e30), r=('bt',), w=('bt',))

        kb.op('act', lambda e: e.activation(out=ebt[:], in_=bt[:], func=AF.Exp), r=('bt',), w=('ebt',))
        ci = 0
        for h in range(4):
            for g, (window, dil) in enumerate(DA_GROUPS):
                L = S // dil
                nqb = L // P
                pad = 64 * dil
                gh = g * 4 + h
                kb.dma('sp', QT[:], projT[16 + gh, :, :], r=('projT',), w=('QT',))
                kb.op('pool', lambda e: e.memset(KTp[:, 0:pad], 0.0), w=('KTp',))
                kb.op('pool', lambda e: e.memset(KTp[:, pad + S:pad + S + pad], 0.0), w=('KTp',))
                kb.op('pool', lambda e: e.memset(VTp[:, 0:pad], 0.0), w=('VTp',))
                kb.op('pool', lambda e: e.memset(VTp[:, pad + S:pad + S + pad], 0.0), w=('VTp',))
                kb.dma('sp', KTp[:, pad:pad + S], projT[28 + gh, :, :], r=('projT',), w=('KTp',), acc=True)
                kb.dma('sp', VTp[:, pad:pad + S], projT[40 + gh, :, :], r=('projT',), w=('VTp',), acc=True)
                nb1 = nqb + 1
                vi = 0
                for r in range(dil):
                    for m in range(nb1):
                        s = vi % 2
                        vi += 1
                        a0 = r + dil * P * m
                        kb.op('pe', lambda e: e.transpose(out=pV[s][:], in_=VTp[:, a0:a0 + dil * (P - 1) + 1:dil],
                                                          identity=ident[:]),
                              r=('VTp', 'ident'), w=(('pV', s),))
                        blk = r * nb1 + m
                        if vi % 2 == 0:
                            kb.op('act', lambda e: e.activation(out=Vtok[:, blk, :], in_=pV[s][:], func=AF.Copy),
                                  r=(('pV', s),), w=('Vtok',))
                        else:
                            kb.op('dve', lambda e: e.tensor_copy(out=Vtok[:, blk, :], in_=pV[s][:]),
                                  r=(('pV', s),), w=('Vtok',))
                qi = 0
                for r in range(dil):
                    for qb in range(nqb):
                        q0 = r + dil * P * qb
                        qap = QT[:, q0:q0 + dil * (P - 1) + 1:dil]
                        b = qi % 2
                        qi += 1
                        for c in range(2):
                            m = qb + c
                            k0 = r + dil * P * m
                            sS = ci % 2
                            sp_ = ci % 4
                            ci += 1
                            kb.op('pe', lambda e: e.matmul(pS[sS][:], lhsT=KTp[:, k0:k0 + dil * (P - 1) + 1:dil],
                                                           rhs=qap, start=True, stop=True),
                                  r=('KTp', 'QT'), w=(('pS', sS),))
                            if c == 0:
                                bty = 2 if qb == 0 else 0
                            else:
                                bty = 3 if qb == nqb - 1 else 1
                            kb.op('act', lambda e: e.activation(out=pt[sp_][:], in_=pS[sS][:], func=AF.Exp),
                                  r=(('pS', sS),), w=(('pt', sp_),))
                            kb.op('pool', lambda e: e.tensor_tensor(out=pt[sp_][:], in0=pt[sp_][:],
                                                                    in1=ebt[:, gh, bty, :], op=ALU.mult),
                                  r=(('pt', sp_), 'ebt'), w=(('pt', sp_),))
                            kb.op('pe', lambda e: e.matmul(pN[b][:], lhsT=Vtok[:, r * nb1 + m, :], rhs=pt[sp_][:],
                                                           start=(c == 0), stop=(c == 1)),
                                  r=('Vtok', ('pt', sp_)), w=(('pN', b),))
                            kb.op('pe', lambda e: e.matmul(pD[b][:], lhsT=onesb[:], rhs=pt[sp_][:],
                                                           start=(c == 0), stop=(c == 1)),
                                  r=('onesb', ('pt', sp_)), w=(('pD', b),))
                        nap = numacc[:, q0:q0 + dil * (P - 1) + 1:dil]
                        dap = denacc[:, q0:q0 + dil * (P - 1) + 1:dil]
                        if g == 0:
                            kb.op('dve', lambda e: e.tensor_copy(out=nap, in_=pN[b][:]), r=(('pN', b),),
                                  w=('numacc',))
                            kb.op('dve', lambda e: e.tensor_copy(out=dap, in_=pD[b][:]), r=(('pD', b),),
                                  w=('denacc',))
                        else:
                            kb.op('dve', lambda e: e.tensor_tensor(out=nap, in0=nap, in1=pN[b][:], op=ALU.add),
                                  r=(('pN', b), 'numacc'), w=('numacc',))
                            kb.op('dve', lambda e: e.tensor_tensor(out=dap, in0=dap, in1=pD[b][:], op=ALU.add),
                                  r=(('pD', b), 'denacc'), w=('denacc',))
            CH = min(2048, S)
            for c0 in range(0, S, CH):
                kb.op('act', lambda e: e.activation(out=denacc[:, c0:c0 + CH], in_=denacc[:, c0:c0 + CH], func=AF.Ln),
                      r=('denacc',), w=('denacc',))
                kb.op('act', lambda e: e.activation(out=denacc[:, c0:c0 + CH], in_=denacc[:, c0:c0 + CH], func=AF.Exp,
                                                    scale=-1.0), r=('denacc',), w=('denacc',))
                kb.op('dve', lambda e: e.tensor_tensor(out=QT[:, c0:c0 + CH], in0=numacc[:, c0:c0 + CH],
                                                       in1=denacc[:, c0:c0 + CH], op=ALU.mult),
                      r=('numacc', 'denacc'), w=('QT',))
            kb.dma('sp', odaT[h, :, :], QT[:], r=('QT',), w=('odaT',), acc=True)


def phase_b0(nc, kb, S, abT, dn_a_log, dn_dt_bias, gam, tokS_d, glb_d):
    C = P
    nch = S // C
    SEG = min(S, 2048)
    nchs = SEG // C
    with ExitStack() as es:
        identf, ident = make_identities(nc, kb, es)
        A = sb(nc, es, "A", [4, SEG], F32)
        B = sb(nc, es, "B", [4, SEG], F32)
        T1 = sb(nc, es, "T1", [4, SEG], F32)
        GF = sb(nc, es, "GF", [4, SEG], F32)
        MK = sb(nc, es, "MK", [4, SEG], F32)
        TR = sb(nc, es, "TR", [40, SEG], F32)
        par = [sb(nc, es, f"par{d}", [4, 4], F32) for d in range(2)]
        GL = sb(nc, es, "GL", [4, nchs], F32)
        sel = [sb(nc, es, f"sel{h}", [4, P], F32) for h in range(4)]
        tok = sb(nc, es, "tok", [P, nch, 40], F32)
        glb = sb(nc, es, "glb", [P, 8, nch], F32)
        pX = [ps(nc, es, f"pX{i}", [P, 512], F32) for i in range(2)]

        for h in range(4):
            kb.op('pool', lambda e: e.memset(sel[h][:], 0.0), w=(('sel', h),))
            kb.op('pool', lambda e: e.affine_select(out=sel[h][:], in_=sel[h][:], pattern=[[0, P]],
                                                    compare_op=ALU.not_equal, fill=1.0, base=-h,
                                                    channel_multiplier=1), r=(('sel', h),), w=(('sel', h),))
        kb.op('pool', lambda e: e.memset(MK[:], 1.0), w=('MK',))
        kb.op('pool', lambda e: e.memset(MK[:, 0:SEG:C], 0.0), w=('MK',))
        for d in range(2):
            kb.dma('sp', par[d][:, 0:1], dn_dt_bias[d:d + 1, :].rearrange("o h -> h o"), r=('dtb',), w=(('par', d),))
            kb.dma('sp', par[d][:, 1:2], dn_a_log[d:d + 1, :].rearrange("o h -> h o"), r=('alog',), w=(('par', d),),
                   acc=True)
            kb.op('act', lambda e: e.activation(out=par[d][:, 2:3], in_=par[d][:, 1:2], func=AF.Exp),
                  r=(('par', d),), w=(('par', d),))
            kb.op('dve', lambda e: e.tensor_scalar(out=par[d][:, 2:3], in0=par[d][:, 2:3], scalar1=-1.0, scalar2=None,
                                                   op0=ALU.mult), r=(('par', d),), w=(('par', d),))
        A3 = A[:, :].rearrange("p (c t) -> p c t", t=C)
        T13 = T1[:, :].rearrange("p (c t) -> p c t", t=C)
        GF3 = GF[:, :].rearrange("p (c t) -> p c t", t=C)
        for s0 in range(0, S, SEG):
            cb = s0 // C
            for d in range(2):
                pk = ('par', d)
                kb.dma('sp', A[:], abT[d * 4:(d + 1) * 4, s0:s0 + SEG], r=('abT',), w=('A',))
                kb.dma('sp', B[:], abT[8 + d * 4:8 + (d + 1) * 4, s0:s0 + SEG], r=('abT',), w=('B',))
                kb.op('dve', lambda e: e.tensor_scalar(out=A[:], in0=A[:], scalar1=par[d][:, 0:1], scalar2=None,
                                                       op0=ALU.add), r=('A', pk), w=('A',))
                kb.op('act', lambda e: e.activation(out=T1[:], in_=A[:], func=AF.Abs), r=('A',), w=('T1',))
                kb.op('act', lambda e: e.activation(out=T1[:], in_=T1[:], func=AF.Exp, scale=-1.0), r=('T1',),
                      w=('T1',))
                kb.op('act', lambda e: e.activation(out=T1[:], in_=T1[:], func=AF.Ln, bias=1.0), r=('T1',), w=('T1',))
                kb.op('dve', lambda e: e.scalar_tensor_tensor(out=A[:], in0=A[:], scalar=0.0, in1=T1[:], op0=ALU.max,
                                                              op1=ALU.add), r=('A', 'T1'), w=('A',))
                kb.op('dve', lambda e: e.tensor_scalar(out=A[:], in0=A[:], scalar1=par[d][:, 2:3], scalar2=None,
                                                       op0=ALU.mult), r=('A', pk), w=('A',))
                kb.op('dve', lambda e: e.tensor_tensor_scan(out=GF[:], data0=MK[:], data1=A[:], initial=0.0,
                                                            op0=ALU.mult, op1=ALU.add), r=('MK', 'A'), w=('GF',))
                tot = GF3[:, :, C - 1:C].to_broadcast([4, nchs, C])
                if d == 1:
                    kb.op('dve', lambda e: e.tensor_tensor(out=A[:], in0=A[:], in1=GF[:], op=ALU.subtract),
                          r=('A', 'GF'), w=('A',))
                    kb.op('dve', lambda e: e.tensor_tensor(out=A3, in0=A3, in1=tot, op=ALU.add), r=('A', 'GF'),
                          w=('A',))
                else:
                    kb.op('dve', lambda e: e.tensor_copy(out=A[:], in_=GF[:]), r=('GF',), w=('A',))
                kb.dma('sp', gam[d, :, s0:s0 + SEG], A[:], r=('A',), w=('gam',), acc=True)
                kb.op('act', lambda e: e.activation(out=B[:], in_=B[:], func=AF.Sigmoid), r=('B',), w=('B',))
                kb.dma('sp', TR[0 + d * 4:0 + d * 4 + 4, :], B[:], r=('B',), w=('TR',), acc=True, sk=('st', 'B'))
                kb.op('dve', lambda e: e.tensor_scalar(out=T1[:], in0=B[:], scalar1=-1.0, scalar2=None, op0=ALU.mult),
                      r=('B',), w=('T1',))
                kb.dma('sp', TR[24 + d * 4:24 + d * 4 + 4, :], T1[:], r=('T1',), w=('TR',), acc=True, sk=('st', 'T1'))
                kb.op('act', lambda e: e.activation(out=B[:], in_=A[:], func=AF.Exp), r=('A',), w=('B',))
                kb.dma('sp', TR[8 + d * 4:8 + d * 4 + 4, :], B[:], r=('B',), w=('TR',), acc=True, sk=('st', 'B'))
                kb.op('dve', lambda e: e.tensor_tensor(out=T13, in0=tot, in1=A3, op=ALU.subtract), r=('A', 'GF'),
                      w=('T1',))
                kb.op('act', lambda e: e.activation(out=T1[:], in_=T1[:], func=AF.Exp), r=('T1',), w=('T1',))
                kb.dma('sp', TR[16 + d * 4:16 + d * 4 + 4, :], T1[:], r=('T1',), w=('TR',), acc=True, sk=('st', 'T1'))
                kb.op('dve', lambda e: e.tensor_scalar(out=T1[:], in0=A[:], scalar1=-1.0, scalar2=None, op0=ALU.mult),
                      r=('A',), w=('T1',))
                kb.dma('sp', TR[32 + d * 4:32 + d * 4 + 4, :], T1[:], r=('T1',), w=('TR',), acc=True, sk=('st', 'T1'))
                kb.op('act', lambda e: e.activation(out=GL[:], in_=GF3[:, :, C - 1], func=AF.Exp), r=('GF',),
                      w=('GL',))
                for h in range(4):
                    kb.op('pe', lambda e: e.matmul(pX[0][:, 0:nchs], lhsT=sel[h][:], rhs=GL[:], start=True, stop=True),
                          r=(('sel', h), 'GL'), w=(('pX', 0),))
                    kb.op('dve', lambda e: e.tensor_copy(out=glb[:, d * 4 + h, cb:cb + nchs], in_=pX[0][:, 0:nchs]),
                          r=(('pX', 0),), w=('glb',))
            for c in range(nchs):
                s = c % 2
                kb.op('pe', lambda e: e.transpose(out=pX[s][:, 0:40], in_=TR[:, c * C:(c + 1) * C],
                                                  identity=identf[0:40, 0:40]),
                      r=('TR', 'identf'), w=(('pX', s),))
                kb.op('act', lambda e: e.activation(out=tok[:, cb + c, :], in_=pX[s][:, 0:40], func=AF.Copy),
                      r=(('pX', s),), w=('tok',))
        kb.dma('sp', tokS_d[:, :, :], tok[:], r=('tok',), w=('tokS_d',))
        kb.dma('sp', glb_d[:, :, :], glb[:], r=('glb',), w=('glb_d',))


def phase_b1(nc, kb, S, projT, dn_conv, dnq, dnk, dnkt, dnvt, bg=None):
    C = P
    nch = S // C
    HS = min(S, 2048)

    def bg_step(k):
        if bg is None:
            return
        for _ in range(k):
            try:
                next(bg)
            except StopIteration:
                return

    with ExitStack() as es:
        identf, ident = make_identities(nc, kb, es)
        onesb = sb(nc, es, "onesb", [P, P], BF16)
        cw = sb(nc, es, "cw", [P, 12, 4], F32)
        xin = [sb(nc, es, f"xin{i}", [P, S + 3], BF16) for i in range(2)]
        acc = sb(nc, es, "acc", [P, HS], F32)
        sq = sb(nc, es, "sq", [P, HS], BF16)
        rn = sb(nc, es, "rn", [P, 512], F32)
        yT = [sb(nc, es, f"yT{i}", [P, S], BF16) for i in range(2)]
        ytok = [sb(nc, es, f"ytok{i}", [P, nch, P], BF16) for i in range(2)]
        pX = [ps(nc, es, f"pX{i}", [P, 512], F32) for i in range(2)]
        pV = [ps(nc, es, f"pV{i}", [P, P], BF16) for i in range(2)]
        kb.op('pool', lambda e: e.memset(onesb[:], 1.0), w=('onesb',))
        for t in range(12):
            kb.dma('sp', cw[:, t, :], dn_conv[:, t * P:(t + 1) * P].rearrange("k c -> c k"), r=('dn_conv',),
                   w=('cw',), acc=True, allow_slow_non_contiguous=True)
        n = 0
        for kind in range(3):
            for h in range(4):
                t = kind * 4 + h
                s = n % 2
                n += 1
                kb.op('pool', lambda e: e.memset(xin[s][:, 0:2], 0.0), w=(('xin', s),))
                kb.op('pool', lambda e: e.memset(xin[s][:, S + 2:S + 3], 0.0), w=(('xin', s),))
                kb.dma('sp', xin[s][:, 2:S + 2], projT[t, :, :], r=('projT',), w=(('xin', s),), acc=True)
                for c0 in range(0, S, HS):
                    kb.op('dve', lambda e: e.tensor_scalar(out=acc[:], in0=xin[s][:, c0:c0 + HS],
                                                           scalar1=cw[:, t, 0:1], scalar2=None, op0=ALU.mult),
                          r=(('xin', s), 'cw'), w=('acc',))
                    for k in range(1, 4):
                        kb.op('dve', lambda e: e.scalar_tensor_tensor(out=acc[:], in0=xin[s][:, c0 + k:c0 + k + HS],
                                                                      scalar=cw[:, t, k:k + 1], in1=acc[:],
                                                                      op0=ALU.mult, op1=ALU.add),
                              r=(('xin', s), 'cw', 'acc'), w=('acc',))
                    if kind == 2:
                        kb.op('act', lambda e: e.activation(out=yT[s][:, c0:c0 + HS], in_=acc[:], func=AF.Silu),
                              r=('acc',), w=(('yT', s),))
                        bg_step(2)
                        continue
                    kb.op('act', lambda e: e.activation(out=acc[:], in_=acc[:], func=AF.Silu), r=('acc',), w=('acc',))
                    kb.op('act', lambda e: e.activation(out=sq[:], in_=acc[:], func=AF.Square), r=('acc',), w=('sq',))
                    for j in range(0, HS, 512):
                        b = (j // 512) % 2
                        kb.op('pe', lambda e: e.matmul(pX[b][:], lhsT=onesb[:], rhs=sq[:, j:j + 512], start=True,
                                                       stop=True), r=('onesb', 'sq'), w=(('pX', b),))
                        kb.op('act', lambda e: e.activation(out=rn[:], in_=pX[b][:], func=AF.Ln, bias=EPS),
                              r=(('pX', b),), w=('rn',))
                        kb.op('act', lambda e: e.activation(out=rn[:], in_=rn[:], func=AF.Exp, scale=-0.5), r=('rn',),
                              w=('rn',))
                        bg_step(1)
                        sc = (128 ** -0.5) if kind == 0 else 1.0
                        kb.op('dve', lambda e: e.scalar_tensor_tensor(out=yT[s][:, c0 + j:c0 + j + 512],
                                                                      in0=acc[:, j:j + 512], scalar=sc, in1=rn[:],
                                                                      op0=ALU.mult, op1=ALU.mult),
                              r=('acc', 'rn'), w=(('yT', s),))
                if kind == 0:
                    kb.dma('pool', dnq[h, :, :], yT[s][:], r=(('yT', s),), w=('dnq',), acc=True)
                    continue
                if kind == 1:
                    kb.dma('pool', dnk[h, :, :], yT[s][:], r=(('yT', s),), w=('dnk',), acc=True)
                for c in range(nch):
                    b = c % 2
                    kb.op('pe', lambda e: e.transpose(out=pV[b][:], in_=yT[s][:, c * C:(c + 1) * C], identity=ident[:]),
                          r=(('yT', s), 'ident'), w=(('pV', b),))
                    if c % 2 == 0:
                        kb.op('act', lambda e: e.activation(out=ytok[s][:, c, :], in_=pV[b][:], func=AF.Copy),
                              r=(('pV', b),), w=(('ytok', s),))
                    else:
                        kb.op('dve', lambda e: e.tensor_copy(out=ytok[s][:, c, :], in_=pV[b][:]),
                              r=(('pV', b),), w=(('ytok', s),))
                dst = dnkt if kind == 1 else dnvt
                kb.dma('pool', dst[h, :, :, :], ytok[s][:], r=(('ytok', s),), w=('dnkt' if kind == 1 else 'dnvt',),
                       acc=True)


def phase_b2(nc, kb, S, projT, dnq, dnk, dnkt, dnvt, gam, tokS_d, glb_d, dn_out_norm, odnT):
    C = P
    nch = S // C
    with ExitStack() as es:
        identf, ident = make_identities(nc, kb, es)
        onesb = sb(nc, es, "onesb", [P, P], BF16)
        MN = [sb(nc, es, f"MN{d}", [P, P], F32) for d in range(2)]
        SMf = sb(nc, es, "SMf", [P, P], F32)
        SM = [sb(nc, es, f"SM{d}", [P, P], BF16) for d in range(2)]
        sel = [sb(nc, es, f"sel{d}", [2, P], F32) for d in range(2)]
        nsel = [sb(nc, es, f"nsel{d}", [2, P], F32) for d in range(2)]
        tokS = sb(nc, es, "tokS", [P, nch, 40], F32)
        glb = sb(nc, es, "glb", [P, 8, nch], F32)
        onorm = sb(nc, es, "onorm", [P, 1], F32)
        qT = sb(nc, es, "qT", [P, S], BF16)
        kT = sb(nc, es, "kT", [P, S], BF16)
        ktok = sb(nc, es, "ktok", [P, nch, P], BF16)
        vtok = sb(nc, es, "vtok", [P, nch, P], BF16)
        G2 = sb(nc, es, "G2", [2, S], F32)
        oacc = sb(nc, es, "oacc", [P, S], F32)
        rn = sb(nc, es, "rn", [P, 512], F32)
        Sf = [sb(nc, es, f"Sf{d}", [P, P], F32) for d in range(2)]
        Sb = [[sb(nc, es, f"Sb{d}{i}", [P, P], BF16) for i in range(2)] for d in range(2)]

        def two(name, dt):
            return [sb(nc, es, f"{name}{i}", [P, P], dt) for i in range(2)]
        DT = two("DT", F32)
        intraT = two("intraT", BF16)
        Mf = two("Mf", F32)
        Pm = [two("Pa", BF16), two("Pb", BF16)]
        Qm = [two("Qa", BF16), two("Qb", BF16)]
        Rm = [two("Ra", BF16), two("Rb", BF16)]
        ub = two("ub", F32)
        kg = two("kg", BF16)
        kd = two("kd", BF16)
        wT = two("wT", BF16)
        egb = two("egb", F32)
        qgT = two("qgT", BF16)
        vnew = two("vnew", BF16)

        banks = [ps(nc, es, f"bk{i}", [P, 512], F32) for i in range(7)]
        bankb = ps(nc, es, "bkb", [P, 1024], BF16)
        for i in range(8):
            kb.excl.add(('bank', i))

        def sub(bank, col):
            return banks[bank][:, col * P:(col + 1) * P]
        PS = []
        for d in range(2):
            a, b, c = 3 * d, 3 * d + 1, 3 * d + 2
            PS.append({
                'pE': (sub(a, 0), ('bank', a)), 'pB': (sub(a, 1), ('bank', a)), 'pG': (sub(a, 2), ('bank', a)),
                'pQK': (sub(a, 3), ('bank', a)),
                'pA0': (sub(b, 0), ('bank', b)), 'pR': (sub(b, 1), ('bank', b)), 'pU': (sub(b, 2), ('bank', b)),
                'pW': (sub(b, 3), ('bank', b)),
                'pA1': (sub(c, 0), ('bank', c)), 'pWS': (sub(c, 1), ('bank', c)), 'pO': (sub(c, 2), ('bank', c)),
                'pS2': (sub(c, 3), ('bank', c)),
                'pTb': (bankb[:, d * P:(d + 1) * P], ('bank', 7)),
            })
        pX = banks[6]

        kb.op('pool', lambda e: e.memset(onesb[:], 1.0), w=('onesb',))
        for d in range(2):
            kb.op('pool', lambda e: e.memset(sel[d][:], 0.0), w=(('sel', d),))
            kb.op('pool', lambda e: e.affine_select(out=sel[d][:], in_=sel[d][:], pattern=[[0, P]],
                                                    compare_op=ALU.not_equal, fill=1.0, base=-d,
                                                    channel_multiplier=1), r=(('sel', d),), w=(('sel', d),))
            kb.op('pool', lambda e: e.memset(nsel[d][:], 0.0), w=(('nsel', d),))
            kb.op('pool', lambda e: e.affine_select(out=nsel[d][:], in_=nsel[d][:], pattern=[[0, P]],
                                                    compare_op=ALU.not_equal, fill=-1.0, base=-d,
                                                    channel_multiplier=1), r=(('nsel', d),), w=(('nsel', d),))
        for d in range(2):
            cm, st = (-1, 1) if d == 0 else (1, -1)
            kb.op('pool', lambda e: e.memset(MN[d][:], 0.0), w=(('MN', d),))
            kb.op('pool', lambda e: e.affine_select(out=MN[d][:], in_=MN[d][:], pattern=[[st, P]],
                                                    compare_op=ALU.is_ge, fill=-1e30, base=0, channel_multiplier=cm),
                  r=(('MN', d),), w=(('MN', d),))
            kb.op('pool', lambda e: e.memset(SMf[:], 1.0), w=('SMf',))
            kb.op('pool', lambda e: e.affine_select(out=SMf[:], in_=SMf[:], pattern=[[st, P]],
                                                    compare_op=ALU.is_gt, fill=0.0, base=0, channel_multiplier=cm),
                  r=('SMf',), w=('SMf',))
            kb.op('dve', lambda e: e.tensor_copy(out=SM[d][:], in_=SMf[:]), r=('SMf',), w=(('SM', d),))
            kb.op('pool', lambda e: e.memset(MN[d][:], 1.0), r=(('MN', d),), w=(('MN', d),))
            kb.op('pool', lambda e: e.affine_select(out=MN[d][:], in_=MN[d][:], pattern=[[st, P]],
                                                    compare_op=ALU.is_ge, fill=0.0, base=0, channel_multiplier=cm),
                  r=(('MN', d),), w=(('MN', d),))
        kb.dma('sp', tokS[:], tokS_d[:, :, :], r=('tokS_d',), w=('tokS',))
        kb.dma('sp', glb[:], glb_d[:, :, :], r=('glb_d',), w=('glb',))
        kb.dma('sp', onorm[:], dn_out_norm[0:1, :].rearrange("o c -> c o"), r=('dn_out_norm',), w=('onorm',))

        def chunk(h, d, c, step):
            s = d
            dh = d * 4 + h
            cs = slice(c * C, (c + 1) * C)
            beta = tokS[:, c, 0 + dh:0 + dh + 1]
            egc = tokS[:, c, 8 + dh:8 + dh + 1]
            ekd = tokS[:, c, 16 + dh:16 + dh + 1]
            nbeta = tokS[:, c, 24 + dh:24 + dh + 1]
            pp = PS[d]
            K = lambda n: pp[n][1] if n in pp else (n, s)
            T = lambda n: pp[n][0]
            negG = tokS[:, c, 32 + dh:32 + dh + 1]
            kb.op('pe', lambda e: e.matmul(T('pB'), lhsT=sel[d][:], rhs=G2[:, cs], start=True, stop=True),
                  r=('G2', ('sel', d)), w=(K('pB'),))
            yield
            kb.op('dve', lambda e: e.tensor_scalar(out=Mf[s][:], in0=T('pB'), scalar1=negG, scalar2=0.0,
                                                   op0=ALU.add, op1=ALU.min), r=(K('pB'), 'tokS'), w=(K('Mf'),))
            kb.op('act', lambda e: e.activation(out=egb[s][:], in_=T('pB'), func=AF.Exp), r=(K('pB'),),
                  w=(K('egb'),))
            yield
            kb.op('act', lambda e: e.activation(out=DT[s][:], in_=Mf[s][:], func=AF.Exp), r=(K('Mf'),),
                  w=(K('DT'),))
            yield
            kb.op('pool', lambda e: e.tensor_tensor(out=DT[s][:], in0=DT[s][:], in1=MN[d][:], op=ALU.mult),
                  r=(K('DT'), ('MN', d)), w=(K('DT'),))
            yield
            kb.op('pe', lambda e: e.matmul(T('pG'), lhsT=kT[:, cs], rhs=kT[:, cs], start=True, stop=True),
                  r=('kT',), w=(K('pG'),))
            kb.op('pe', lambda e: e.matmul(T('pQK'), lhsT=kT[:, cs], rhs=qT[:, cs], start=True, stop=True),
                  r=('kT', 'qT'), w=(K('pQK'),))
            yield
            kb.op('dve', lambda e: e.scalar_tensor_tensor(out=Mf[s][:], in0=T('pG'), scalar=beta, in1=DT[s][:],
                                                          op0=ALU.mult, op1=ALU.mult),
                  r=(K('pG'), K('DT'), 'tokS'), w=(K('Mf'),))
            kb.op('dve', lambda e: e.tensor_tensor(out=intraT[s][:], in0=T('pQK'), in1=DT[s][:], op=ALU.mult),
                  r=(K('pQK'), K('DT')), w=(K('intraT'),))
            yield
            P0, Q0, R0 = Pm[0][s], Qm[0][s], Rm[0][s]
            kb.op('pool', lambda e: e.tensor_tensor(out=P0[:], in0=Mf[s][:], in1=SM[d][:], op=ALU.mult),
                  r=(K('Mf'), ('SM', d)), w=(K('P0'),))
            kb.op('pool', lambda e: e.tensor_tensor(out=R0[:], in0=ident[:], in1=P0[:], op=ALU.subtract),
                  r=('ident', K('P0')), w=(K('R0'),))
            yield
            kb.op('pe', lambda e: e.transpose(out=T('pTb'), in_=P0[:], identity=ident[:]),
                  r=(K('P0'), 'ident'), w=(K('pTb'),))
            yield
            kb.op('act', lambda e: e.activation(out=Q0[:], in_=T('pTb'), func=AF.Copy), r=(K('pTb'),),
                  w=(K('Q0'),))
            yield
            for k in range(1, 7):
                a, b = (k - 1) % 2, k % 2
                Pp, Qp, Rp = Pm[a][s], Qm[a][s], Rm[a][s]
                Pn, Qn, Rn = Pm[b][s], Qm[b][s], Rm[b][s]
                pa, pb_ = f'P{a}', f'P{b}'
                qa, qb_ = f'Q{a}', f'Q{b}'
                ra, rb_ = f'R{a}', f'R{b}'
                kb.op('pe', lambda e: e.matmul(T('pA0'), lhsT=Pp[:], rhs=Qp[:], start=True, stop=True),
                      r=(K(pa), K(qa)), w=(K('pA0'),))
                if k < 6:
                    kb.op('pe', lambda e: e.matmul(T('pA1'), lhsT=Qp[:], rhs=Pp[:], start=True, stop=True),
                          r=(K(pa), K(qa)), w=(K('pA1'),))
                yield
                kb.op('act', lambda e: e.activation(out=Qn[:], in_=T('pA0'), func=AF.Copy), r=(K('pA0'),),
                      w=(K(qb_),))
                if k < 6:
                    kb.op('dve', lambda e: e.tensor_copy(out=Pn[:], in_=T('pA1')), r=(K('pA1'),), w=(K(pb_),))
                yield
                kb.op('pe', lambda e: e.matmul(T('pR'), lhsT=Qn[:], rhs=Rp[:], start=True, stop=True),
                      r=(K(qb_), K(ra)), w=(K('pR'),))
                yield
                kb.op('dve', lambda e: e.tensor_tensor(out=Rn[:], in0=Rp[:], in1=T('pR'), op=ALU.add),
                      r=(K(ra), K('pR')), w=(K(rb_),))
                yield
            TT = Rm[0][s]
            kT_ = K('R0')
            kb.op('pool', lambda e: e.tensor_scalar(out=kg[s][:], in0=ktok[:, c, :], scalar1=egc, scalar2=None,
                                                    op0=ALU.mult), r=('ktok', 'tokS'), w=(K('kg'),))
            kb.op('pool', lambda e: e.tensor_scalar(out=kd[s][:], in0=ktok[:, c, :], scalar1=ekd, scalar2=None,
                                                    op0=ALU.mult), r=('ktok', 'tokS'), w=(K('kd'),))
            yield
            kb.op('pe', lambda e: e.matmul(T('pU'), lhsT=TT[:], rhs=vtok[:, c, :], start=True, stop=True),
                  r=(kT_, 'vtok'), w=(K('pU'),))
            kb.op('pe', lambda e: e.matmul(T('pW'), lhsT=kg[s][:], rhs=TT[:], start=True, stop=True),
                  r=(K('kg'), kT_), w=(K('pW'),))
            yield
            kb.op('act', lambda e: e.activation(out=ub[s][:], in_=T('pU'), func=AF.Copy, scale=beta),
                  r=(K('pU'), 'tokS'), w=(K('ub'),))
            kb.op('act', lambda e: e.activation(out=wT[s][:], in_=T('pW'), func=AF.Copy), r=(K('pW'),),
                  w=(K('wT'),))
            kb.op('dve', lambda e: e.tensor_tensor(out=qgT[s][:], in0=qT[:, cs], in1=egb[s][:], op=ALU.mult),
                  r=('qT', K('egb')), w=(K('qgT'),))
            yield
            so, sn = step % 2, (step + 1) % 2
            kb.op('pe', lambda e: e.matmul(T('pWS'), lhsT=wT[s][:], rhs=Sb[d][so][:], start=True, stop=True),
                  r=(K('wT'), ('Sb', d, so)), w=(K('pWS'),))
            yield
            kb.op('dve', lambda e: e.scalar_tensor_tensor(out=vnew[s][:], in0=T('pWS'), scalar=nbeta,
                                                          in1=ub[s][:], op0=ALU.mult, op1=ALU.add),
                  r=(K('pWS'), K('ub'), 'tokS'), w=(K('vnew'),))
            yield
            kb.op('pe', lambda e: e.matmul(T('pO'), lhsT=Sb[d][so][:], rhs=qgT[s][:], start=True, stop=False),
                  r=(('Sb', d, so), K('qgT')), w=(K('pO'),))
            kb.op('pe', lambda e: e.matmul(T('pO'), lhsT=vnew[s][:], rhs=intraT[s][:], start=False, stop=True),
                  r=(K('vnew'), K('intraT')), w=(K('pO'),))
            kb.op('pe', lambda e: e.matmul(T('pS2'), lhsT=kd[s][:], rhs=vnew[s][:], start=True, stop=True),
                  r=(K('kd'), K('vnew')), w=(K('pS2'),))
            yield
            first = (c < nch // 2) if d == 0 else (c >= nch // 2)
            if first:
                kb.op('act', lambda e: e.activation(out=oacc[:, cs], in_=T('pO'), func=AF.Copy),
                      r=(K('pO'),), w=(('oacc', c),))
            else:
                kb.op('dve', lambda e: e.tensor_tensor(out=oacc[:, cs], in0=oacc[:, cs], in1=T('pO'),
                                                       op=ALU.add), r=(K('pO'), ('oacc', c)), w=(('oacc', c),))
            kb.op('dve', lambda e: e.scalar_tensor_tensor(out=Sf[d][:], in0=Sf[d][:], scalar=glb[:, dh, c:c + 1],
                                                          in1=T('pS2'), op0=ALU.mult, op1=ALU.add),
                  r=(('Sf', d), 'glb', K('pS2')), w=(('Sf', d),))
            yield
            kb.op('act', lambda e: e.activation(out=Sb[d][sn][:], in_=Sf[d][:], func=AF.Copy), r=(('Sf', d),),
                  w=(('Sb', d, sn),))
            yield

        for h in range(4):
            kb.dma('sp', qT[:], dnq[h, :, :], r=('dnq',), w=('qT',))
            kb.dma('sp', kT[:], dnk[h, :, :], r=('dnk',), w=('kT',))
            kb.dma('sp', ktok[:], dnkt[h, :, :, :], r=('dnkt',), w=('ktok',))
            kb.dma('sp', vtok[:], dnvt[h, :, :, :], r=('dnvt',), w=('vtok',))
            kb.dma('sp', G2[0:1, :], gam[0, h:h + 1, :], r=('gam',), w=('G2',))
            kb.dma('sp', G2[1:2, :], gam[1, h:h + 1, :], r=('gam',), w=('G2',), acc=True)
            for d in range(2):
                kb.op('pool', lambda e: e.memset(Sf[d][:], 0.0), w=(('Sf', d),))
                kb.op('pool', lambda e: e.memset(Sb[d][0][:], 0.0), w=(('Sb', d, 0),))
            for step in range(nch):
                g0 = chunk(h, 0, step, step)
                g1 = chunk(h, 1, nch - 1 - step, step)
                done0 = done1 = False
                while not (done0 and done1):
                    if not done0:
                        try:
                            next(g0)
                        except StopIteration:
                            done0 = True
                    if not done1:
                        try:
                            next(g1)
                        except StopIteration:
                            done1 = True
            allo = tuple(('oacc', c) for c in range(nch))
            kb.dma('sp', kT[:], projT[12 + h, :, :], r=('projT',), w=('kT',))
            for j in range(0, S, 512):
                js = slice(j, j + 512)
                ok_ = tuple(('oacc', c) for c in range(j // C, (j + 512) // C))
                kb.op('act', lambda e: e.activation(out=qT[:, js], in_=oacc[:, js], func=AF.Square), r=ok_,
                      w=('qT',))
                kb.op('pe', lambda e: e.matmul(pX[:], lhsT=onesb[:], rhs=qT[:, js], start=True, stop=True),
                      r=('onesb', 'qT'), w=(('bank', 6),))
                kb.op('act', lambda e: e.activation(out=rn[:], in_=pX[:], func=AF.Ln, scale=1.0 / P, bias=EPS),
                      r=(('bank', 6),), w=('rn',))
                kb.op('act', lambda e: e.activation(out=rn[:], in_=rn[:], func=AF.Exp, scale=-0.5), r=('rn',),
                      w=('rn',))
                kb.op('dve', lambda e: e.scalar_tensor_tensor(out=rn[:], in0=rn[:], scalar=onorm[:, 0:1],
                                                              in1=oacc[:, js], op0=ALU.mult, op1=ALU.mult),
                      r=('rn', 'onorm') + ok_, w=('rn',))
                kb.op('dve', lambda e: e.tensor_tensor(out=qT[:, js], in0=rn[:], in1=kT[:, js], op=ALU.mult),
                      r=('rn', 'kT'), w=('qT',))
            kb.dma('sp', odnT[h, :, :], qT[:], r=('qT',), w=('odnT',), acc=True)


def phase_d(nc, kb, S, x, projT, odnT, odaT, w_bdn, w_bda, w_out, norm_ffn, peer_wq, sub_keys, hbuf, xn2d, scores_d):
    ST = 512
    with ExitStack() as es:
        identf, ident = make_identities(nc, kb, es)
        Wbdn = sb(nc, es, "Wbdn", [P, 4, D], BF16)
        Wbda = sb(nc, es, "Wbda", [P, 4, D], BF16)
        Wout = sb(nc, es, "Wout", [P, KC, D], BF16)
        Wq = sb(nc, es, "Wq", [P, KC, 2048], BF16)
        skT = sb(nc, es, "skT", [P, 16, P], BF16)
        gffn = sb(nc, es, "gffn", [P, D], F32)
        stage = [sb(nc, es, f"stage{i}", [P, D], F32) for i in range(2)]
        odn = sb(nc, es, "odn", [P, 4, ST], BF16)
        oda = sb(nc, es, "oda", [P, 4, ST], BF16)
        gdn = sb(nc, es, "gdn", [P, 8, ST], BF16)
        gda = sb(nc, es, "gda", [P, 8, ST], BF16)
        mergedT = sb(nc, es, "mergedT", [P, 8, ST], BF16)
        t1 = sb(nc, es, "t1", [P, ST], F32)
        t2 = sb(nc, es, "t2", [P, ST], F32)
        xt = sb(nc, es, "xt", [P, D], F32)
        ht = [sb(nc, es, f"ht{i}", [P, D], F32) for i in range(2)]
        junk = sb(nc, es, "junk", [P, D], BF16)
        ss = sb(nc, es, "ss", [P, 2], F32)
        xn2 = [sb(nc, es, f"xn2{i}", [P, D], BF16) for i in range(2)]
        xn2T = sb(nc, es, "xn2T", [P, KC, ST], BF16)
        qhT = sb(nc, es, "qhT", [P, 16, ST], BF16)
        sc = [sb(nc, es, f"sc{i}", [P, 2048], F32) for i in range(2)]
        pM = [ps(nc, es, f"pM{i}", [P, ST], F32) for i in range(2)]
        pH = [ps(nc, es, f"pH{i}", [P, ST], F32) for i in range(2)]
        pT = ps(nc, es, "pT", [P, KC, P], BF16)
        pQ = ps(nc, es, "pQ", [P, ST], F32)
        pSc = [ps(nc, es, f"pSc{i}", [P, ST], F32) for i in range(2)]

        kb.dma('sp', gffn[:], norm_ffn[0:1, :].partition_broadcast(P), r=('norm_ffn',), w=('gffn',))
        n = [0]

        def loadw(dst, src, nk, ncols, key):
            for kc in range(nk):
                for c0 in range(0, ncols, D):
                    s_ = n[0] % 2
                    n[0] += 1
                    kb.dma('sp', stage[s_][:], src[kc * P:(kc + 1) * P, c0:c0 + D], r=(key + '_d',), w=(('stage', s_),))
                    if s_ == 0:
                        kb.op('act', lambda e: e.activation(out=dst[:, kc, c0:c0 + D], in_=stage[s_][:], func=AF.Copy),
                              r=(('stage', s_),), w=(key,))
                    else:
                        kb.op('dve', lambda e: e.tensor_copy(out=dst[:, kc, c0:c0 + D], in_=stage[s_][:]),
                              r=(('stage', s_),), w=(key,))
        loadw(Wbdn, w_bdn, 4, D, 'Wbdn')
        loadw(Wbda, w_bda, 4, D, 'Wbda')
        loadw(Wout, w_out, KC, D, 'Wout')
        loadw(Wq, peer_wq, KC, 2048, 'Wq')
        for hc in range(16):
            s_ = n[0] % 2
            n[0] += 1
            kb.dma('sp', stage[s_][:, 0:P], sub_keys[hc, :, :], r=('sub_keys',), w=(('stage', s_),))
            kb.op('pe', lambda e: e.transpose(out=pQ[:, 0:P], in_=stage[s_][:, 0:P], identity=identf[:]),
                  r=(('stage', s_), 'identf'), w=('pQ',))
            kb.op('dve', lambda e: e.tensor_copy(out=skT[:, hc, :], in_=pQ[:, 0:P]), r=('pQ',), w=('skT',))

        ti = 0
        for st in range(S // ST):
            tk = slice(st * ST, (st + 1) * ST)
            kb.dma('sp', odn[:], odnT[:, :, tk].rearrange("c p t -> p c t"), r=('odnT',), w=('odn',))
            kb.dma('sp', oda[:], odaT[:, :, tk].rearrange("c p t -> p c t"), r=('odaT',), w=('oda',))
            kb.dma('sp', gdn[:], projT[52:60, :, tk].rearrange("c p t -> p c t"), r=('projT',), w=('gdn',))
            kb.dma('sp', gda[:], projT[60:68, :, tk].rearrange("c p t -> p c t"), r=('projT',), w=('gda',))
            for m in range(8):
                for kc in range(4):
                    kb.op('pe', lambda e: e.matmul(pM[0][:], lhsT=Wbdn[:, kc, m * P:(m + 1) * P], rhs=odn[:, kc, :],
                                                   start=(kc == 0), stop=(kc == 3)), r=('Wbdn', 'odn'), w=(('pM', 0),))
                for kc in range(4):
                    kb.op('pe', lambda e: e.matmul(pM[1][:], lhsT=Wbda[:, kc, m * P:(m + 1) * P], rhs=oda[:, kc, :],
                                                   start=(kc == 0), stop=(kc == 3)), r=('Wbda', 'oda'), w=(('pM', 1),))
                kb.op('dve', lambda e: e.tensor_tensor(out=t1[:], in0=pM[0][:], in1=gdn[:, m, :], op=ALU.mult),
                      r=(('pM', 0), 'gdn'), w=('t1',))
                kb.op('dve', lambda e: e.tensor_tensor(out=t2[:], in0=pM[1][:], in1=gda[:, m, :], op=ALU.mult),
                      r=(('pM', 1), 'gda'), w=('t2',))
                kb.op('pool', lambda e: e.tensor_tensor(out=mergedT[:, m, :], in0=t1[:], in1=t2[:], op=ALU.add),
                      r=('t1', 't2'), w=('mergedT',))
            for j in range(ST // P):
                s_ = ti % 2
                ti += 1
                t0 = st * ST + j * P
                kb.dma('sp', xt[:], x[t0:t0 + P, :], r=('x',), w=('xt',))
                for nh in range(2):
                    for mc in range(8):
                        kb.op('pe', lambda e: e.matmul(pH[nh][:], lhsT=mergedT[:, mc, j * P:(j + 1) * P],
                                                       rhs=Wout[:, mc, nh * ST:(nh + 1) * ST], start=(mc == 0),
                                                       stop=(mc == 7)), r=('mergedT', 'Wout'), w=(('pH', nh),))
                    kb.op('dve', lambda e: e.tensor_tensor(out=ht[s_][:, nh * ST:(nh + 1) * ST],
                                                           in0=xt[:, nh * ST:(nh + 1) * ST], in1=pH[nh][:], op=ALU.add),
                          r=('xt', ('pH', nh)), w=(('ht', s_),))
                kb.dma('pool', hbuf[t0:t0 + P, :], ht[s_][:], r=(('ht', s_),), w=('hbuf',), acc=True)
                kb.op('act', lambda e: e.activation(out=junk[:], in_=ht[s_][:], func=AF.Square, scale=D ** -0.5,
                                                    accum_out=ss[:, 0:1]), r=(('ht', s_),), w=('junk', 'ss'))
                kb.op('dve', lambda e: e.tensor_scalar(out=ss[:, 1:2], in0=ss[:, 0:1], scalar1=EPS, scalar2=None,
                                                       op0=ALU.add), r=('ss',), w=('ss',))
                kb.op('act', lambda e: e.activation(out=ss[:, 1:2], in_=ss[:, 1:2], func=AF.Sqrt), r=('ss',), w=('ss',))
                kb.op('dve', lambda e: e.reciprocal(out=ss[:, 1:2], in_=ss[:, 1:2]), r=('ss',), w=('ss',))
                kb.op('dve', lambda e: e.scalar_tensor_tensor(out=xn2[s_][:], in0=ht[s_][:], scalar=ss[:, 1:2],
                                                              in1=gffn[:], op0=ALU.mult, op1=ALU.mult),
                      r=(('ht', s_), 'ss', 'gffn'), w=(('xn2', s_),))
                kb.dma('pool', xn2d[t0:t0 + P, :], xn2[s_][:], r=(('xn2', s_),), w=('xn2d',), acc=True)
                for kc in range(KC):
                    kb.op('pe', lambda e: e.transpose(out=pT[:, kc, :], in_=xn2[s_][:, kc * P:(kc + 1) * P],
                                                      identity=ident[:]), r=(('xn2', s_), 'ident'), w=('pT',))
                kb.op('act', lambda e: e.activation(out=xn2T[:, :, j * P:(j + 1) * P], in_=pT[:], func=AF.Copy),
                      r=('pT',), w=('xn2T',))
            for hc in range(16):
                for kc in range(KC):
                    kb.op('pe', lambda e: e.matmul(pQ[:], lhsT=Wq[:, kc, hc * P:(hc + 1) * P], rhs=xn2T[:, kc, :],
                                                   start=(kc == 0), stop=(kc == KC - 1)), r=('Wq', 'xn2T'), w=('pQ',))
                if hc % 2 == 0:
                    kb.op('act', lambda e: e.activation(out=qhT[:, hc, :], in_=pQ[:], func=AF.Copy), r=('pQ',),
                          w=('qhT',))
                else:
                    kb.op('dve', lambda e: e.tensor_copy(out=qhT[:, hc, :], in_=pQ[:]), r=('pQ',), w=('qhT',))
            for j in range(ST // P):
                t0 = st * ST + j * P
                s_ = j % 2
                for bk in range(4):
                    b = bk % 2
                    for q in range(4):
                        hc = bk * 4 + q
                        kb.op('pe', lambda e: e.matmul(pSc[b][:, q * P:(q + 1) * P], lhsT=qhT[:, hc, j * P:(j + 1) * P],
                                                       rhs=skT[:, hc, :], start=True, stop=True),
                              r=('qhT', 'skT'), w=(('pSc', b),))
                    if bk % 2 == 0:
                        kb.op('act', lambda e: e.activation(out=sc[s_][:, bk * ST:(bk + 1) * ST], in_=pSc[b][:],
                                                            func=AF.Copy), r=(('pSc', b),), w=(('sc', s_),))
                    else:
                        kb.op('dve', lambda e: e.tensor_copy(out=sc[s_][:, bk * ST:(bk + 1) * ST], in_=pSc[b][:]),
                              r=(('pSc', b),), w=(('sc', s_),))
                kb.dma('pool', scores_d[t0:t0 + P, :], sc[s_][:], r=(('sc', s_),), w=('scores_d',), acc=True)


def phase_p_gen(nc, kb, es, peer_u, peer_v, uvb_d):
    NSL = 4
    RB = 2
    st_, ob_ = es
    n = 0
    for (src, dst, nm) in ((peer_u, uvb_d[:, 0:D], 'uvb_d'), (peer_v, uvb_d[:, D:2 * D], 'uvb_d')):
        for r0 in range(0, 16384, RB * P):
            s_ = n % NSL
            n += 1
            kb.dma('sp', st_[s_][:], src[r0:r0 + RB * P, :].rearrange("(a p) d -> p a d", p=P), r=('peer_tab',),
                   w=(('pst', s_),))
            if n % 2 == 0:
                kb.op('act', lambda e: e.activation(out=ob_[s_][:], in_=st_[s_][:], func=AF.Copy),
                      r=(('pst', s_),), w=(('pob', s_),))
            else:
                kb.op('pool', lambda e: e.tensor_copy(out=ob_[s_][:], in_=st_[s_][:]), r=(('pst', s_),),
                      w=(('pob', s_),))
            kb.dma('act', dst[r0:r0 + RB * P, :].rearrange("(a p) d -> p a d", p=P), ob_[s_][:],
                   r=(('pob', s_),), w=(nm,), acc=True)
            yield


def phase_e(nc, kb, S, hbuf, xn2d, scores_d, uvb_d, norm_final, y):
    NS = 4
    NB = 6
    GC = 0.7978845608028654
    nt = S // P
    with ExitStack() as es:
        identf, ident = make_identities(nc, kb, es)
        gfin = sb(nc, es, "gfin", [P, D], F32)
        act4 = [sb(nc, es, f"act4{i}", [P, NS], F32) for i in range(2)]
        g14 = [sb(nc, es, f"g14{i}", [P, NS], F32) for i in range(2)]
        g24 = [sb(nc, es, f"g24{i}", [P, NS], F32) for i in range(2)]
        c44 = [sb(nc, es, f"c44{i}", [P, NS], F32) for i in range(2)]
        dg = [sb(nc, es, f"dg{i}", [P, P], BF16) for i in range(4)]
        pO = [[ps(nc, es, f"pO{i}{j}", [P, 512], F32) for j in range(2)] for i in range(2)]
        sc = [sb(nc, es, f"sc{i}", [P, 16, P], F32) for i in range(2)]
        wk = sb(nc, es, "wk", [P, 256], F32)
        sv = sb(nc, es, "sv", [P, 16, 16], F32)
        si = sb(nc, es, "si", [P, 16, 16], U32)
        sif = sb(nc, es, "sif", [P, 16, 16], F32)
        cand = sb(nc, es, "cand", [P, 8, 256], F32)
        cv = sb(nc, es, "cv", [P, 8, 16], F32)
        ci = sb(nc, es, "ci", [P, 8, 16], U32)
        cu = sb(nc, es, "cu", [P, 8, 16], U32)
        ikf = sb(nc, es, "ikf", [P, 8, 16], F32)
        jkf = sb(nc, es, "jkf", [P, 8, 16], F32)
        iot_i = sb(nc, es, "iot_i", [P, 16], I32)
        iot = sb(nc, es, "iot", [P, 16], F32)
        iot3 = sb(nc, es, "iot3", [P, 16, 16], F32)
        oh = sb(nc, es, "oh", [P, 16, 16], F32)
        i1 = sb(nc, es, "i1", [P, 8, 16], F32)
        i2 = sb(nc, es, "i2", [P, 8, 16], F32)
        eidf = sb(nc, es, "eidf", [P, P], F32)
        eid = [sb(nc, es, f"eid{i}", [P, P], U32) for i in range(2)]
        gates = [sb(nc, es, f"gates{i}", [P, 8, 16], F32) for i in range(2)]
        gsum = sb(nc, es, "gsum", [P, 8], F32)
        act_ = sb(nc, es, "act_", [P, P], F32)
        g1 = sb(nc, es, "g1", [P, P], F32)
        g2 = sb(nc, es, "g2", [P, P], F32)
        coef = sb(nc, es, "coef", [P, P], F32)
        xn2 = [sb(nc, es, f"xn2{i}", [P, D], BF16) for i in range(2)]
        ht = [sb(nc, es, f"ht{i}", [P, D], F32) for i in range(2)]
        oacc = sb(nc, es, "oacc", [P, D], F32)
        junk = sb(nc, es, "junk", [P, D], F32)
        prod = [sb(nc, es, f"prod{i}", [P, D], BF16) for i in range(4)]
        ident4 = sb(nc, es, "ident4", [P, NS, P], BF16)
        dg4 = [sb(nc, es, f"dg4{i}", [P, NS, P], BF16) for i in range(3)]
        ss = sb(nc, es, "ss", [P, 2], F32)
        yt = [sb(nc, es, f"yt{i}", [P, D], F32) for i in range(2)]
        GB = [sb(nc, es, f"GB{i}", [P, NS, 2 * D], BF16) for i in range(NB)]

        kb.dma('sp', gfin[:], norm_final[0:1, :].partition_broadcast(P), r=('norm_final',), w=('gfin',))
        kb.op('dve', lambda e: e.tensor_copy(out=ident4[:], in_=ident[:, :].unsqueeze(1).to_broadcast([P, NS, P])),
              r=('ident',), w=('ident4',))
        kb.op('pool', lambda e: e.iota(iot_i[:], pattern=[[1, 16]], base=0, channel_multiplier=0), w=('iot_i',))
        kb.op('dve', lambda e: e.tensor_copy(out=iot[:], in_=iot_i[:]), r=('iot_i',), w=('iot',))
        kb.op('dve', lambda e: e.tensor_copy(out=iot3[:], in_=iot[:, :].unsqueeze(1).to_broadcast([P, 16, 16])),
              r=('iot',), w=('iot3',))

        def topk_gen(t):
            s_ = t % 2
            t0 = t * P
            kb.dma('sp', sc[s_][:], scores_d[t0:t0 + P, :].rearrange("p (a b) -> p a b", b=P), r=('scores_d',),
                   w=(('sc', s_),))
            kb.dma('sp', xn2[s_][:], xn2d[t0:t0 + P, :], r=('xn2d',), w=(('xn2', s_),))
            kb.dma('sp', ht[s_][:], hbuf[t0:t0 + P, :], r=('hbuf',), w=(('ht', s_),))
            for hc in range(16):
                src = sc[s_][:, hc, :]
                kb.op('dve', lambda e: e.max(out=sv[:, hc, 0:8], in_=src), r=(('sc', s_),), w=('sv',))
                kb.op('dve', lambda e: e.max_index(out=si[:, hc, 0:8], in_max=sv[:, hc, 0:8], in_values=src),
                      r=(('sc', s_), 'sv'), w=('si',))
                kb.op('dve', lambda e: e.match_replace(out=wk[:, 0:P], in_to_replace=sv[:, hc, 0:8], in_values=src,
                                                       imm_value=-1e30), r=(('sc', s_), 'sv'), w=('wk',))
                kb.op('dve', lambda e: e.max(out=sv[:, hc, 8:16], in_=wk[:, 0:P]), r=('wk',), w=('sv',))
                kb.op('dve', lambda e: e.max_index(out=si[:, hc, 8:16], in_max=sv[:, hc, 8:16], in_values=wk[:, 0:P]),
                      r=('wk', 'sv'), w=('si',))
                yield
            kb.op('dve', lambda e: e.tensor_copy(out=sif[:], in_=si[:]), r=('si',), w=('sif',))
            for h in range(8):
                c3 = cand[:, h, :].rearrange("p (i j) -> p i j", j=16)
                kb.op('dve', lambda e: e.tensor_copy(out=c3, in_=sv[:, 2 * h, :].unsqueeze(2).to_broadcast([P, 16, 16])),
                      r=('sv',), w=('cand',))
                kb.op('dve', lambda e: e.tensor_tensor(out=c3, in0=c3,
                                                       in1=sv[:, 2 * h + 1, :].unsqueeze(1).to_broadcast([P, 16, 16]),
                                                       op=ALU.add), r=('sv', 'cand'), w=('cand',))
                src = cand[:, h, :]
                kb.op('dve', lambda e: e.max(out=cv[:, h, 0:8], in_=src), r=('cand',), w=('cv',))
                kb.op('dve', lambda e: e.max_index(out=ci[:, h, 0:8], in_max=cv[:, h, 0:8], in_values=src),
                      r=('cand', 'cv'), w=('ci',))
                kb.op('dve', lambda e: e.match_replace(out=wk[:], in_to_replace=cv[:, h, 0:8], in_values=src,
                                                       imm_value=-1e30), r=('cand', 'cv'), w=('wk',))
                kb.op('dve', lambda e: e.max(out=cv[:, h, 8:16], in_=wk[:]), r=('wk',), w=('cv',))
                kb.op('dve', lambda e: e.max_index(out=ci[:, h, 8:16], in_max=cv[:, h, 8:16], in_values=wk[:]),
                      r=('wk', 'cv'), w=('ci',))
                yield
            kb.op('dve', lambda e: e.tensor_scalar(out=cu[:], in0=ci[:], scalar1=4, scalar2=None,
                                                   op0=ALU.logical_shift_right), r=('ci',), w=('cu',))
            kb.op('dve', lambda e: e.tensor_copy(out=ikf[:], in_=cu[:]), r=('cu',), w=('ikf',))
            kb.op('dve', lambda e: e.tensor_scalar(out=cu[:], in0=ci[:], scalar1=15, scalar2=None,
                                                   op0=ALU.bitwise_and), r=('ci',), w=('cu',))
            kb.op('dve', lambda e: e.tensor_copy(out=jkf[:], in_=cu[:]), r=('cu',), w=('jkf',))
            for h in range(8):
                for (kf, half, dst) in ((ikf, 0, i1), (jkf, 1, i2)):
                    kb.op('dve', lambda e: e.tensor_tensor(out=oh[:], in0=iot3[:],
                                                           in1=kf[:, h, :].unsqueeze(2).to_broadcast([P, 16, 16]),
                                                           op=ALU.is_equal), r=('iot3', 'ikf', 'jkf'), w=('oh',))
                    kb.op('dve', lambda e: e.tensor_tensor(out=oh[:], in0=oh[:],
                                                           in1=sif[:, 2 * h + half, :].unsqueeze(1).to_broadcast([P, 16, 16]),
                                                           op=ALU.mult), r=('oh', 'sif'), w=('oh',))
                    kb.op('dve', lambda e: e.tensor_reduce(out=dst[:, h, :], in_=oh[:], axis=AX.X, op=ALU.add),
                          r=('oh',), w=('i1', 'i2'))
                yield
            kb.op('dve', lambda e: e.scalar_tensor_tensor(out=eidf[:], in0=i1[:].rearrange("p a b -> p (a b)"),
                                                          scalar=128.0, in1=i2[:].rearrange("p a b -> p (a b)"),
                                                          op0=ALU.mult, op1=ALU.add), r=('i1', 'i2'), w=('eidf',))
            kb.op('dve', lambda e: e.tensor_copy(out=eid[s_][:], in_=eidf[:]), r=('eidf',), w=(('eid', s_),))
            gt = gates[s_]
            gk = ('gates', s_)
            kb.op('dve', lambda e: e.tensor_tensor(out=gt[:], in0=cv[:], in1=cv[:, :, 0:1].to_broadcast([P, 8, 16]),
                                                   op=ALU.subtract), r=('cv',), w=(gk,))
            kb.op('act', lambda e: e.activation(out=gt[:], in_=gt[:], func=AF.Exp), r=(gk,), w=(gk,))
            kb.op('dve', lambda e: e.tensor_reduce(out=gsum[:], in_=gt[:], axis=AX.X, op=ALU.add), r=(gk,),
                  w=('gsum',))
            kb.op('dve', lambda e: e.reciprocal(out=gsum[:], in_=gsum[:]), r=('gsum',), w=('gsum',))
            kb.op('dve', lambda e: e.tensor_tensor(out=gt[:], in0=gt[:],
                                                   in1=gsum[:, :].unsqueeze(2).to_broadcast([P, 8, 16]), op=ALU.mult),
                  r=(gk, 'gsum'), w=(gk,))

        def topk(t):
            for _ in topk_gen(t):
                pass

        gi = [0]

        def gather(e_, s0):
            b = gi[0] % NB
            gi[0] += 1
            for q in range(NS):
                kb.dma('pool', GB[b][:, q, :], uvb_d[:, :], r=(('eid', e_),), w=(('GB', b),), acc=(q > 0),
                       indirect=bass.IndirectOffsetOnAxis(ap=eid[e_][:, s0 + q:s0 + q + 1], axis=0))
            return b

        nb_t = P // NS
        di = [0]

        def stage_a(t, k, idx):
            s_ = t % 2
            s0 = k * NS
            b = gather(s_, s0)
            kk = idx % 2
            a4 = act4[kk]
            for q in range(NS):
                pq = q % 4
                kb.op('dve', lambda e: e.tensor_tensor(out=prod[pq][:], in0=GB[b][:, q, 0:D], in1=xn2[s_][:],
                                                       op=ALU.mult), r=(('GB', b), ('xn2', s_)), w=(('prod', pq),))
                kb.op('act', lambda e: e.activation(out=junk[:], in_=prod[pq][:], func=AF.Copy,
                                                    accum_out=a4[:, q:q + 1]),
                      r=(('prod', pq),), w=(('a4', kk, q),))
            return b

        def stage_b(t, k, idx, b):
            s_ = t % 2
            s0 = k * NS
            kk = idx % 2
            a4, g1, g2, c4 = act4[kk], g14[kk], g24[kk], c44[kk]
            ak = tuple(('a4', kk, q) for q in range(NS))
            kb.op('dve', lambda e: e.tensor_tensor(out=g1[:], in0=a4[:], in1=a4[:], op=ALU.mult), r=ak,
                  w=(('g1', kk),))
            kb.op('dve', lambda e: e.tensor_scalar(out=g1[:], in0=g1[:], scalar1=0.044715, scalar2=1.0,
                                                   op0=ALU.mult, op1=ALU.add), r=(('g1', kk),), w=(('g1', kk),))
            kb.op('dve', lambda e: e.tensor_tensor(out=g1[:], in0=g1[:], in1=a4[:], op=ALU.mult),
                  r=(('g1', kk),) + ak, w=(('g1', kk),))
            kb.op('act', lambda e: e.activation(out=g2[:], in_=g1[:], func=AF.Tanh, scale=GC), r=(('g1', kk),),
                  w=(('g2', kk),))
            kb.op('dve', lambda e: e.scalar_tensor_tensor(out=g2[:], in0=g2[:], scalar=1.0, in1=a4[:],
                                                          op0=ALU.add, op1=ALU.mult),
                  r=(('g2', kk),) + ak, w=(('g2', kk),))
            gflat = gates[s_][:].rearrange("p a b -> p (a b)")
            kb.op('dve', lambda e: e.scalar_tensor_tensor(out=c4[:], in0=g2[:], scalar=0.5,
                                                          in1=gflat[:, s0:s0 + NS], op0=ALU.mult, op1=ALU.mult),
                  r=(('g2', kk), ('gates', s_)), w=(('c4', kk),))
            r_ = di[0] % 3
            di[0] += 1
            kb.op('dve', lambda e: e.tensor_tensor(out=dg4[r_][:], in0=ident4[:],
                                                   in1=c4[:, :].unsqueeze(2).to_broadcast([P, NS, P]), op=ALU.mult),
                  r=('ident4', ('c4', kk)), w=(('dg4', r_),))
            for q in range(NS):
                slot = s0 + q
                for nh in range(2):
                    kb.op('pe', lambda e: e.matmul(pO[s_][nh][:], lhsT=dg4[r_][:, q, :],
                                                   rhs=GB[b][:, q, D + nh * 512:D + (nh + 1) * 512],
                                                   start=(slot == 0), stop=(slot == P - 1)),
                          r=(('dg4', r_), ('GB', b)), w=(('pO', s_, nh),))

        def tile_end(t):
            s_ = t % 2
            t0 = t * P
            for nh in range(2):
                kb.op('dve', lambda e: e.tensor_tensor(out=oacc[:, nh * 512:(nh + 1) * 512], in0=pO[s_][nh][:],
                                                       in1=ht[s_][:, nh * 512:(nh + 1) * 512], op=ALU.add),
                      r=(('pO', s_, nh), ('ht', s_)), w=('oacc',))
            kb.op('act', lambda e: e.activation(out=junk[:], in_=oacc[:], func=AF.Square, scale=D ** -0.5,
                                                accum_out=ss[:, 0:1]), r=('oacc',), w=('junk', 'ss'))
            kb.op('dve', lambda e: e.tensor_scalar(out=ss[:, 1:2], in0=ss[:, 0:1], scalar1=EPS, scalar2=None,
                                                   op0=ALU.add), r=('ss',), w=('ss',))
            kb.op('act', lambda e: e.activation(out=ss[:, 1:2], in_=ss[:, 1:2], func=AF.Sqrt), r=('ss',), w=('ss',))
            kb.op('dve', lambda e: e.reciprocal(out=ss[:, 1:2], in_=ss[:, 1:2]), r=('ss',), w=('ss',))
            kb.op('dve', lambda e: e.scalar_tensor_tensor(out=yt[s_][:], in0=oacc[:], scalar=ss[:, 1:2], in1=gfin[:],
                                                          op0=ALU.mult, op1=ALU.mult), r=('oacc', 'ss', 'gfin'),
                  w=(('yt', s_),))
            kb.dma('sp', y[t0:t0 + P, :], yt[s_][:], r=(('yt', s_),), w=('y',), acc=True)

        batches = [(t, k) for t in range(nt) for k in range(nb_t)]
        topk(0)
        tkg = None
        cur_b = stage_a(batches[0][0], batches[0][1], 0)
        for idx, (t, k) in enumerate(batches):
            nxt_b = None
            if idx + 1 < len(batches):
                nxt_b = stage_a(batches[idx + 1][0], batches[idx + 1][1], idx + 1)
            stage_b(t, k, idx, cur_b)
            if k == 2 and t + 1 < nt:
                tkg = topk_gen(t + 1)
            if tkg is not None and k >= 2:
                for _ in range(2):
                    try:
                        next(tkg)
                    except StopIteration:
                        tkg = None
                        break
            if k == nb_t - 2 and tkg is not None:
                for _ in tkg:
                    pass
                tkg = None
            if k == nb_t - 1:
                tile_end(t)
            cur_b = nxt_b


def build(S, dbg=False, phases="pabcde"):
    nc = bass.Bass("TRN2", target_bir_lowering=False)
    okind = "ExternalOutput" if dbg else "Internal"
    nch = S // P

    def inp(name, shape, dt=F32):
        return nc.dram_tensor(name, list(shape), dt, kind="ExternalInput").ap()

    def scr(name, shape, dt, out=False):
        return nc.dram_tensor(name, list(shape), dt, kind=okind if not out else "ExternalOutput").ap()

    x = inp("x", [S, D])
    norm_mix = inp("norm_mix", [1, D])
    w_in = inp("w_in", [D, NW])
    rel_bias = inp("rel_bias", [32, 12])
    c_ohv = inp("c_ohv", [33, 6, 256])
    dn_conv = inp("dn_conv", [4, 1536])
    dn_a_log = inp("dn_a_log", [2, 4])
    dn_dt_bias = inp("dn_dt_bias", [2, 4])
    dn_out_norm = inp("dn_out_norm", [1, P])
    w_bdn = inp("w_branch_dn", [512, D])
    w_bda = inp("w_branch_da", [512, D])
    w_out = inp("w_out", [D, D])
    norm_ffn = inp("norm_ffn", [1, D])
    peer_wq = inp("peer_wq", [D, 2048])
    sub_keys = inp("peer_sub_keys", [16, P, P])
    peer_u = inp("peer_u", [16384, D])
    peer_v = inp("peer_v", [16384, D])
    norm_final = inp("norm_final", [1, D])
    projT = scr("projT", [68, P, S], BF16)
    abT = scr("abT", [16, S], F32)
    hv = scr("hv", [24, 256], F32)
    odaT = scr("odaT", [4, P, S], BF16)
    gam = scr("gam", [2, 4, S], F32)
    tokS_d = scr("tokS_d", [P, nch, 40], F32)
    glb_d = scr("glb_d", [P, 8, nch], F32)
    dnq = scr("dnq", [4, P, S], BF16)
    dnk = scr("dnk", [4, P, S], BF16)
    dnkt = scr("dnkt", [4, P, nch, P], BF16)
    dnvt = scr("dnvt", [4, P, nch, P], BF16)
    odnT = scr("odnT", [4, P, S], BF16)
    hbuf = scr("hbuf", [S, D], F32)
    xn2d = scr("xn2d", [S, D], BF16)
    scores_d = scr("scores_d", [S, 2048], F32)
    y = scr("y", [S, D], F32, out=True)
    uvb_d = scr("uvb_d", [16384, 2 * D], BF16)
    with ExitStack() as es:
        kb = KB(nc, es)
        phase_a(nc, kb, S, x, w_in, norm_mix, projT, abT)
        kb.barrier()
        if "b" in phases:
            phase_b0(nc, kb, S, abT, dn_a_log, dn_dt_bias, gam, tokS_d, glb_d)
            kb.barrier()
            with ExitStack() as es_p:
                pg = None
                if "p" in phases:
                    p_tiles = ([sb(nc, es_p, f"pst{i}", [P, 2, D], F32) for i in range(4)],
                               [sb(nc, es_p, f"pob{i}", [P, 2, D], BF16) for i in range(4)])
                    pg = phase_p_gen(nc, kb, p_tiles, peer_u, peer_v, uvb_d)
                phase_b1(nc, kb, S, projT, dn_conv, dnq, dnk, dnkt, dnvt, bg=pg)
                if pg is not None:
                    for _ in pg:
                        pass
                kb.barrier()
            phase_b2(nc, kb, S, projT, dnq, dnk, dnkt, dnvt, gam, tokS_d, glb_d, dn_out_norm, odnT)
            kb.barrier()
        if "c" in phases:
            phase_c(nc, kb, S, projT, rel_bias, c_ohv, hv, odaT)
            kb.barrier()
        if "d" in phases:
            phase_d(nc, kb, S, x, projT, odnT, odaT, w_bdn, w_bda, w_out, norm_ffn, peer_wq, sub_keys, hbuf, xn2d,
                    scores_d)
            kb.barrier()
        if "e" in phases:
            phase_e(nc, kb, S, hbuf, xn2d, scores_d, uvb_d, norm_final, y)
            kb.barrier()
        print("ninst", kb.ninst, "nsem", kb.nsem)
    return nc


def make_in_map(inputs, b, S):
    f = lambda a: np.ascontiguousarray(np.asarray(a, dtype=np.float32))
    return {
        "x": f(inputs["x"][b, :S]),
        "norm_mix": f(inputs["norm_mix"]).reshape(1, D),
        "w_in": f(inputs["w_in"]).reshape(D, NW),
        "rel_bias": f(inputs["rel_bias"]),
        "c_ohv": make_ohv(),
        "dn_conv": f(inputs["dn_conv"]).reshape(4, 1536),
        "dn_a_log": f(inputs["dn_a_log"]).reshape(2, 4),
        "dn_dt_bias": f(inputs["dn_dt_bias"]).reshape(2, 4),
        "dn_out_norm": f(inputs["dn_out_norm"]).reshape(1, P),
        "w_branch_dn": f(inputs["w_branch_dn"]).reshape(512, D),
        "w_branch_da": f(inputs["w_branch_da"]).reshape(512, D),
        "w_out": f(inputs["w_out"]).reshape(D, D),
        "norm_ffn": f(inputs["norm_ffn"]).reshape(1, D),
        "peer_wq": f(inputs["peer_wq"]).reshape(D, 2048),
        "peer_sub_keys": f(inputs["peer_sub_keys"]).reshape(16, P, P),
        "peer_u": f(inputs["peer_u"]).reshape(16384, D),
        "peer_v": f(inputs["peer_v"]).reshape(16384, D),
        "norm_final": f(inputs["norm_final"]).reshape(1, D),
    }


def kernel(**inputs):
    B, S = inputs["x"].shape[0], inputs["x"].shape[1]
    nc = build(S)
    base = make_in_map(inputs, 0, S)
    in_maps = []
    for b in range(B):
        m = dict(base)
        m["x"] = np.ascontiguousarray(np.asarray(inputs["x"][b], dtype=np.float32))
        in_maps.append(m)
    res = run_bass_kernel_spmd(nc, in_maps, core_ids=list(range(B)))
    return np.stack([np.asarray(r["y"], dtype=np.float32) for r in res.results], axis=0)
```

```python
import math
from contextlib import ExitStack

import numpy as np
import concourse.bass as bass
import concourse.mybir as mybir
from concourse.bass_utils import run_bass_kernel_spmd

F32 = mybir.dt.float32
BF16 = mybir.dt.bfloat16
I32 = mybir.dt.int32
U32 = mybir.dt.uint32
AF = mybir.ActivationFunctionType
ALU = mybir.AluOpType
AX = mybir.AxisListType

P = 128
D = 1024
KC = D // P
NW = 8720
EPS = 1e-6
SEM_LIMIT = 24000


class KB:
    def __init__(self, nc, es):
        self.nc, self.es = nc, es
        self.E = {'pe': nc.tensor, 'act': nc.scalar, 'dve': nc.vector, 'pool': nc.gpsimd, 'sp': nc.sync}
        self.esem = {}
        self.seen = {e: {} for e in self.E}
        self.W = {}
        self.R = {}
        self.dsem = {}
        self.nsem = 0
        self.ninst = {e: 0 for e in self.E}
        self.excl = set()
        self.dma_sids = set()

    def newsem(self, name):
        self.nsem += 1
        return self.es.enter_context(self.nc.semaphore(f"{name}_{self.nsem}"))

    def _esem(self, eng):
        s = self.esem.get(eng)
        if s is None or s[1] >= SEM_LIMIT:
            s = [self.newsem("e" + eng), 0]
            self.esem[eng] = s
        return s

    @staticmethod
    def _merge(deps, d):
        for sid, (sem, val) in d.items():
            if sid not in deps or deps[sid][1] < val:
                deps[sid] = (sem, val)

    def _wait(self, eng, deps):
        for sid, (sem, val) in deps.items():
            if self.seen[eng].get(sid, 0) < val:
                self.E[eng].wait_ge(sem, val)
                self.seen[eng][sid] = val

    def _deps(self, r, w, skip_sid=None):
        deps = {}
        for k in r:
            self._merge(deps, self.W.get(k, {}))
        for k in w:
            self._merge(deps, self.W.get(k, {}))
            self._merge(deps, self.R.get(k, {}))
        if skip_sid is not None:
            deps.pop(skip_sid, None)
        return deps

    def op(self, eng, fn, r=(), w=()):
        if self.excl:
            w = tuple(w) + tuple(k for k in r if k in self.excl and k not in w)
        deps = self._deps(r, w)
        if eng == 'pe':
            s0 = self.esem.get('pe')
            for sid in list(deps):
                if deps[sid][0] is (s0[0] if s0 else None):
                    deps.pop(sid)
        self._wait(eng, deps)
        inst = fn(self.E[eng])
        s = self._esem(eng)
        s[1] += 1
        inst.then_inc(s[0], 1)
        self.ninst[eng] += 1
        sid = id(s[0])
        for k in r:
            self.R.setdefault(k, {})[sid] = (s[0], s[1])
        for k in w:
            self.W[k] = {sid: (s[0], s[1])}
            self.R[k] = {}
        return inst

    def dma(self, q, out, in_, r=(), w=(), acc=False, indirect=None, sk=None, **kw):
        if sk is None:
            sk = ('st', r[0]) if (str(out.space).endswith('DRAM') and r) else w[0]
        ds = self.dsem.get(sk)
        if ds is None or ds[1] >= SEM_LIMIT:
            ds = [self.newsem("d"), 0]
            self.dsem[sk] = ds
            self.dma_sids.add(id(ds[0]))
        sid = id(ds[0])
        deps = {}
        for k in r:
            self._merge(deps, self.W.get(k, {}))
        for k in w:
            if not acc:
                self._merge(deps, self.W.get(k, {}))
            else:
                self._merge(deps, {i: v for i, v in self.W.get(k, {}).items() if i not in self.dma_sids})
            self._merge(deps, self.R.get(k, {}))
        self._wait(q, deps)
        if indirect is None:
            inst = self.E[q].dma_start(out=out, in_=in_, **kw)
        else:
            inst = self.E[q].indirect_dma_start(out=out, out_offset=None, in_=in_, in_offset=indirect)
        ds[1] += 16
        inst.then_inc(ds[0], 16)
        self.ninst[q] += 1
        for k in r:
            self.R.setdefault(k, {})[sid] = (ds[0], ds[1])
        for k in w:
            if acc:
                self.W.setdefault(k, {})[sid] = (ds[0], ds[1])
            else:
                self.W[k] = {sid: (ds[0], ds[1])}
                self.R[k] = {}
        return inst

    def barrier(self):
        deps = {}
        for e, s in self.esem.items():
            if s[1] > 0:
                deps[id(s[0])] = (s[0], s[1])
        for k, s in self.dsem.items():
            if s[1] > 0:
                deps[id(s[0])] = (s[0], s[1])
        for e in self.E:
            self._wait(e, deps)

    def finish(self, keys, eng='sp'):
        deps = self._deps(keys, ())
        self._wait(eng, deps)


_UID = [0]


def sb(nc, es, name, shape, dt):
    _UID[0] += 1
    return es.enter_context(nc.sbuf_tensor(f"{name}_{_UID[0]}", list(shape), dt))


def ps(nc, es, name, shape, dt):
    _UID[0] += 1
    return es.enter_context(nc.psum_tensor(f"{name}_{_UID[0]}", list(shape), dt))


def col_tiles():
    tiles = []
    c = 0
    for nm, wdt, fn, sc in (("dq", 512, None, 1.0), ("dk", 512, None, 1.0), ("dv", 512, None, 1.0),
                            ("dz", 512, AF.Silu, 1.0)):
        for i in range(wdt // P):
            tiles.append((nm, c, P, fn, sc))
            c += P
    tiles.append(("ab", c, 16, None, 1.0))
    c += 16
    for nm, wdt, fn, sc in (("aq", 1536, None, 128 ** -0.5), ("ak", 1536, None, 1.0), ("av", 1536, None, 1.0),
                            ("gdn", 1024, AF.Sigmoid, 1.0), ("gda", 1024, AF.Sigmoid, 1.0)):
        for i in range(wdt // P):
            tiles.append((nm, c, P, fn, sc))
            c += P
    assert c == NW
    return tiles


def phase_a(nc, kb, S, x, w_in, norm_mix, projT, abT):
    tiles = col_tiles()
    ST = 512
    with ExitStack() as es:
        w_bf = sb(nc, es, "w_bf", [P, KC, NW], BF16)
        gmix = sb(nc, es, "gmix", [P, D], F32)
        ident = sb(nc, es, "ident", [P, P], BF16)
        identf = sb(nc, es, "identf", [P, P], F32)
        xt = [sb(nc, es, f"xt{i}", [P, D], F32) for i in range(2)]
        junk = sb(nc, es, "junk", [P, D], BF16)
        ss = [sb(nc, es, f"ss{i}", [P, 2], F32) for i in range(2)]
        xn = [sb(nc, es, f"xn{i}", [P, D], BF16) for i in range(2)]
        xnT = [sb(nc, es, f"xnT{i}", [P, KC, ST], BF16) for i in range(2)]
        ot = [sb(nc, es, f"ot{i}", [P, ST], BF16) for i in range(4)]
        otf = sb(nc, es, "otf", [16, ST], F32)
        pT = [ps(nc, es, f"pT{i}", [P, KC, P], BF16) for i in range(2)]
        pM = [ps(nc, es, f"pM{i}", [P, ST], F32) for i in range(4)]

        kb.dma('sp', gmix[:], norm_mix[0:1, :].partition_broadcast(P), r=('norm_mix',), w=('gmix',))
        kb.op('pool', lambda e: e.memset(identf[:], 0.0), w=('identf',))
        kb.op('pool', lambda e: e.affine_select(out=identf[:], in_=identf[:], pattern=[[-1, P]],
                                                compare_op=ALU.not_equal, fill=1.0, base=0, channel_multiplier=1),
              r=('identf',), w=('identf',))
        kb.op('dve', lambda e: e.tensor_copy(out=ident[:], in_=identf[:]), r=('identf',), w=('ident',))

        NPC = 10
        pw = NW // NPC
        assert pw <= D
        n = 0
        for kc in range(KC):
            for pc in range(NPC):
                s = n % 2
                kb.dma('sp', xt[s][:, 0:pw], w_in[kc * P:(kc + 1) * P, pc * pw:(pc + 1) * pw],
                       r=('w_in',), w=(('xt', s),))
                eng = 'act' if n % 2 == 0 else 'dve'
                if eng == 'act':
                    kb.op('act', lambda e: e.activation(out=w_bf[:, kc, pc * pw:(pc + 1) * pw], in_=xt[s][:, 0:pw],
                                                        func=AF.Copy), r=(('xt', s),), w=('w_bf',))
                else:
                    kb.op('dve', lambda e: e.tensor_copy(out=w_bf[:, kc, pc * pw:(pc + 1) * pw], in_=xt[s][:, 0:pw]),
                          r=(('xt', s),), w=('w_bf',))
                n += 1

        nst = S // ST
        ti = 0
        oi = 0
        pi = 0
        for st in range(nst):
            xs = st % 2
            for j in range(ST // P):
                s = ti % 2
                t0 = st * ST + j * P
                kb.dma('sp', xt[s][:], x[t0:t0 + P, :], r=('x',), w=(('xt', s),))
                kb.op('act', lambda e: e.activation(out=junk[:], in_=xt[s][:], func=AF.Square, scale=D ** -0.5,
                                                    accum_out=ss[s][:, 0:1]),
                      r=(('xt', s),), w=('junk', ('ss', s)))
                kb.op('dve', lambda e: e.tensor_scalar(out=ss[s][:, 1:2], in0=ss[s][:, 0:1], scalar1=EPS,
                                                       scalar2=None, op0=ALU.add),
                      r=(('ss', s),), w=(('ss', s),))
                kb.op('act', lambda e: e.activation(out=ss[s][:, 1:2], in_=ss[s][:, 1:2], func=AF.Sqrt),
                      r=(('ss', s),), w=(('ss', s),))
                kb.op('dve', lambda e: e.reciprocal(out=ss[s][:, 1:2], in_=ss[s][:, 1:2]),
                      r=(('ss', s),), w=(('ss', s),))
                kb.op('dve', lambda e: e.scalar_tensor_tensor(out=xn[s][:], in0=xt[s][:], scalar=ss[s][:, 1:2],
                                                              in1=gmix[:], op0=ALU.mult, op1=ALU.mult),
                      r=(('xt', s), ('ss', s), 'gmix'), w=(('xn', s),))
                for kc in range(KC):
                    kb.op('pe', lambda e: e.transpose(out=pT[s][:, kc, :], in_=xn[s][:, kc * P:(kc + 1) * P],
                                                      identity=ident[:]),
                          r=(('xn', s), 'ident'), w=(('pT', s),))
                kb.op('act', lambda e: e.activation(out=xnT[xs][:, :, j * P:(j + 1) * P], in_=pT[s][:],
                                                    func=AF.Copy),
                      r=(('pT', s),), w=(('xnT', xs),))
                ti += 1
            for (nm, c0, cw, fn, sc) in tiles:
                pb = pi % 4
                pi += 1
                for kc in range(KC):
                    kb.op('pe', lambda e: e.matmul(pM[pb][0:cw, :], lhsT=w_bf[:, kc, c0:c0 + cw],
                                                   rhs=xnT[xs][:, kc, :], start=(kc == 0), stop=(kc == KC - 1)),
                          r=('w_bf', ('xnT', xs)), w=(('pM', pb),))
                if nm == "ab":
                    kb.op('dve', lambda e: e.tensor_copy(out=otf[:], in_=pM[pb][0:16, :]),
                          r=(('pM', pb),), w=('otf',))
                    kb.dma('pool', abT[:, st * ST:(st + 1) * ST], otf[:], r=('otf',), w=('abT',), acc=True)
                    continue
                o = oi % 4
                oi += 1
                if fn is not None:
                    kb.op('act', lambda e: e.activation(out=ot[o][:], in_=pM[pb][:], func=fn),
                          r=(('pM', pb),), w=(('ot', o),))
                elif oi % 2 == 0:
                    kb.op('act', lambda e: e.activation(out=ot[o][:], in_=pM[pb][:], func=AF.Copy, scale=sc),
                          r=(('pM', pb),), w=(('ot', o),))
                else:
                    kb.op('dve', lambda e: e.tensor_scalar(out=ot[o][:], in0=pM[pb][:], scalar1=sc, scalar2=None,
                                                           op0=ALU.mult),
                          r=(('pM', pb),), w=(('ot', o),))
                ct = (c0 if c0 < 2048 else c0 - 16) // P
                kb.dma('pool', projT[ct, :, st * ST:(st + 1) * ST], ot[o][:], r=(('ot', o),), w=('projT',), acc=True)


DA_GROUPS = ((128, 1), (512, 4), (2048, 16))


def t5_bucket_np(rel):
    half = 16
    max_exact = 8
    n = np.abs(rel)
    large = max_exact + (np.log(np.maximum(n, 1) / max_exact) / math.log(1024 / max_exact)
                         * (half - max_exact)).astype(np.int64)
    large = np.minimum(large, half - 1)
    return np.where(rel > 0, half, 0) + np.where(n < max_exact, n, large)


def make_ohv():
    out = np.zeros((33, 6, 256), np.float32)
    for g, (window, dil) in enumerate(DA_GROUPS):
        for ty in range(2):
            for j in range(256):
                d = 127 - j
                rel = d - 64 if ty == 0 else d + 64
                if j < 255 and abs(rel) <= 64:
                    b = int(t5_bucket_np(np.array([rel * dil]))[0])
                    out[b, g * 2 + ty, j] = 1.0
                else:
                    out[32, g * 2 + ty, j] = -1e30
    return out


def make_identities(nc, kb, es):
    identf = sb(nc, es, "identf", [P, P], F32)
    ident = sb(nc, es, "ident", [P, P], BF16)
    kb.op('pool', lambda e: e.memset(identf[:], 0.0), w=('identf',))
    kb.op('pool', lambda e: e.affine_select(out=identf[:], in_=identf[:], pattern=[[-1, P]],
                                            compare_op=ALU.not_equal, fill=1.0, base=0, channel_multiplier=1),
          r=('identf',), w=('identf',))
    kb.op('dve', lambda e: e.tensor_copy(out=ident[:], in_=identf[:]), r=('identf',), w=('ident',))
    return identf, ident


def phase_c(nc, kb, S, projT, rel_bias, c_ohv, hv, odaT):
    PADMAX = 64 * 16
    with ExitStack() as es:
        identf, ident = make_identities(nc, kb, es)
        Jf = sb(nc, es, "Jf", [P, P], F32)
        onesb = sb(nc, es, "onesb", [P, P], BF16)
        tblx = sb(nc, es, "tblx", [33, 12], F32)
        ohv = sb(nc, es, "ohv", [33, 6, 256], F32)
        hvs = sb(nc, es, "hvs", [4, 256], F32)
        hk = [sb(nc, es, f"hk{i}", [P, P], F32) for i in range(2)]
        bt = sb(nc, es, "bt", [P, 12, 4, P], BF16)
        QT = sb(nc, es, "QT", [P, S], BF16)
        KTp = sb(nc, es, "KTp", [P, S + 2 * PADMAX], BF16)
        VTp = sb(nc, es, "VTp", [P, S + 2 * PADMAX], BF16)
        Vtok = sb(nc, es, "Vtok", [P, 80, P], BF16)
        numacc = sb(nc, es, "numacc", [P, S], F32)
        denacc = sb(nc, es, "denacc", [P, S], F32)
        pt = [sb(nc, es, f"pt{i}", [P, P], BF16) for i in range(4)]
        pS = [ps(nc, es, f"pS{i}", [P, P], F32) for i in range(2)]
        pN = [ps(nc, es, f"pN{i}", [P, P], F32) for i in range(2)]
        pD = [ps(nc, es, f"pD{i}", [P, P], F32) for i in range(2)]
        pV = [ps(nc, es, f"pV{i}", [P, P], BF16) for i in range(2)]

        kb.op('pool', lambda e: e.memset(Jf[:], 0.0), w=('Jf',))
        kb.op('pool', lambda e: e.affine_select(out=Jf[:], in_=Jf[:], pattern=[[1, P]],
                                                compare_op=ALU.not_equal, fill=1.0, base=-(P - 1),
                                                channel_multiplier=1), r=('Jf',), w=('Jf',))
        kb.op('pool', lambda e: e.memset(onesb[:], 1.0), w=('onesb',))
        kb.op('pool', lambda e: e.memset(tblx[:], 1.0), w=('tblx',))
        kb.dma('sp', tblx[0:32, :], rel_bias[:, :], r=('rel_bias',), w=('tblx',))
        kb.dma('sp', ohv[:], c_ohv[:, :, :], r=('c_ohv',), w=('ohv',))
        n = 0
        for g in range(3):
            for ty in range(2):
                kb.op('pe', lambda e: e.matmul(pN[0][0:4, :], lhsT=tblx[:, g * 4:(g + 1) * 4],
                                               rhs=ohv[:, g * 2 + ty, 0:P], start=True, stop=True),
                      r=('tblx', 'ohv'), w=(('pN', 0),))
                kb.op('pe', lambda e: e.matmul(pN[1][0:4, :], lhsT=tblx[:, g * 4:(g + 1) * 4],
                                               rhs=ohv[:, g * 2 + ty, P:2 * P], start=True, stop=True),
                      r=('tblx', 'ohv'), w=(('pN', 1),))
                kb.op('dve', lambda e: e.tensor_copy(out=hvs[:, 0:P], in_=pN[0][0:4, :]), r=(('pN', 0),), w=('hvs',))
                kb.op('dve', lambda e: e.tensor_copy(out=hvs[:, P:2 * P], in_=pN[1][0:4, :]), r=(('pN', 1),),
                      w=('hvs',))
                kb.dma('sp', hv[(g * 2 + ty) * 4:(g * 2 + ty) * 4 + 4, :], hvs[:], r=('hvs',), w=('hv',))
                for h in range(4):
                    s = n % 2
                    n += 1
                    src = bass.AP(tensor=hv.tensor, offset=((g * 2 + ty) * 4 + h) * 256, ap=[[1, P], [1, P]])
                    kb.dma('sp', hk[s][:], src, r=('hv',), w=(('hk', s),))
                    kb.op('pe', lambda e: e.matmul(pS[s][:], lhsT=Jf[:], rhs=hk[s][:], start=True, stop=True),
                          r=('Jf', ('hk', s)), w=(('pS', s),))
                    gh = g * 4 + h
                    kb.op('dve', lambda e: e.tensor_copy(out=bt[:, gh, ty, :], in_=pS[s][:]), r=(('pS', s),),
                          w=('bt',))
                    kb.op('dve', lambda e: e.tensor_copy(out=bt[:, gh, 2 + ty, :], in_=pS[s][:]), r=(('pS', s),),
                          w=('bt',))
                    if ty == 0:
                        kb.op('pool', lambda e: e.memset(bt[0:64, gh, 2, :], -1e30), r=('bt',), w=('bt',))
                    else:
                        kb.op('pool', lambda e: e.memset(bt[64:128, gh, 3, :], -1e30), r=('bt',), w=('bt',))

        ci = 0
        for h in range(4):
            for g, (window, dil) in enumerate(DA_GROUPS):
                L = S // dil
                nqb = L // P
                pad = 64 * dil
                gh = g * 4 + h
                kb.dma('sp', QT[:], projT[16 + gh, :, :], r=('projT',), w=('QT',))
                kb.op('pool', lambda e: e.memset(KTp[:, 0:pad], 0.0), w=('KTp',))
                kb.op('pool', lambda e: e.memset(KTp[:, pad + S:pad + S + pad], 0.0), w=('KTp',))
                kb.op('pool', lambda e: e.memset(VTp[:, 0:pad], 0.0), w=('VTp',))
                kb.op('pool', lambda e: e.memset(VTp[:, pad + S:pad + S + pad], 0.0), w=('VTp',))
                kb.dma('sp', KTp[:, pad:pad + S], projT[28 + gh, :, :], r=('projT',), w=('KTp',), acc=True)
                kb.dma('sp', VTp[:, pad:pad + S], projT[40 + gh, :, :], r=('projT',), w=('VTp',), acc=True)
                nb1 = nqb + 1
                vi = 0
                for r in range(dil):
                    for m in range(nb1):
                        s = vi % 2
                        vi += 1
                        a0 = r + dil * P * m
                        kb.op('pe', lambda e: e.transpose(out=pV[s][:], in_=VTp[:, a0:a0 + dil * (P - 1) + 1:dil],
                                                          identity=ident[:]),
                              r=('VTp', 'ident'), w=(('pV', s),))
                        blk = r * nb1 + m
                        if vi % 2 == 0:
                            kb.op('act', lambda e: e.activation(out=Vtok[:, blk, :], in_=pV[s][:], func=AF.Copy),
                                  r=(('pV', s),), w=('Vtok',))
                        else:
                            kb.op('dve', lambda e: e.tensor_copy(out=Vtok[:, blk, :], in_=pV[s][:]),
                                  r=(('pV', s),), w=('Vtok',))
                items = []
                qi = 0
                for r in range(dil):
                    for qb in range(nqb):
                        b = qi % 2
                        qi += 1
                        for c in range(2):
                            items.append((r, qb, c, b))

                def st1(it):
                    nonlocal ci
                    r, qb, c, b = it
                    q0 = r + dil * P * qb
                    qap = QT[:, q0:q0 + dil * (P - 1) + 1:dil]
                    k0 = r + dil * P * (qb + c)
                    sS = ci % 2
                    sp_ = ci % 4
                    ci += 1
                    kb.op('pe', lambda e: e.matmul(pS[sS][:], lhsT=KTp[:, k0:k0 + dil * (P - 1) + 1:dil],
                                                   rhs=qap, start=True, stop=False),
                          r=('KTp', 'QT'), w=(('pS', sS),))
                    if c == 0:
                        bty = 2 if qb == 0 else 0
                    else:
                        bty = 3 if qb == nqb - 1 else 1
                    kb.op('pe', lambda e: e.matmul(pS[sS][:], lhsT=ident[:], rhs=bt[:, gh, bty, :],
                                                   start=False, stop=True),
                          r=('ident', 'bt'), w=(('pS', sS),))
                    kb.op('act', lambda e: e.activation(out=pt[sp_][:], in_=pS[sS][:], func=AF.Exp),
                          r=(('pS', sS),), w=(('pt', sp_),))
                    return sp_

                def st2(it, sp_):
                    r, qb, c, b = it
                    m = qb + c
                    kb.op('pe', lambda e: e.matmul(pN[b][:], lhsT=Vtok[:, r * nb1 + m, :], rhs=pt[sp_][:],
                                                   start=(c == 0), stop=(c == 1)),
                          r=('Vtok', ('pt', sp_)), w=(('pN', b),))
                    kb.op('pe', lambda e: e.matmul(pD[b][:], lhsT=onesb[:], rhs=pt[sp_][:],
                                                   start=(c == 0), stop=(c == 1)),
                          r=('onesb', ('pt', sp_)), w=(('pD', b),))
                    if c != 1:
                        return
                    q0 = r + dil * P * qb
                    nap = numacc[:, q0:q0 + dil * (P - 1) + 1:dil]
                    dap = denacc[:, q0:q0 + dil * (P - 1) + 1:dil]
                    if g == 0:
                        kb.op('dve', lambda e: e.tensor_copy(out=nap, in_=pN[b][:]), r=(('pN', b),),
                              w=('numacc',))
                        kb.op('dve', lambda e: e.tensor_copy(out=dap, in_=pD[b][:]), r=(('pD', b),),
                              w=('denacc',))
                    else:
                        kb.op('dve', lambda e: e.tensor_tensor(out=nap, in0=nap, in1=pN[b][:], op=ALU.add),
                              r=(('pN', b), 'numacc'), w=('numacc',))
                        kb.op('dve', lambda e: e.tensor_tensor(out=dap, in0=dap, in1=pD[b][:], op=ALU.add),
                              r=(('pD', b), 'denacc'), w=('denacc',))

                cur = st1(items[0])
                for i_, it in enumerate(items):
                    nxt = st1(items[i_ + 1]) if i_ + 1 < len(items) else None
                    st2(it, cur)
                    cur = nxt
            CH = min(2048, S)
            for c0 in range(0, S, CH):
                kb.op('act', lambda e: e.activation(out=denacc[:, c0:c0 + CH], in_=denacc[:, c0:c0 + CH], func=AF.Ln),
                      r=('denacc',), w=('denacc',))
                kb.op('act', lambda e: e.activation(out=denacc[:, c0:c0 + CH], in_=denacc[:, c0:c0 + CH], func=AF.Exp,
                                                    scale=-1.0), r=('denacc',), w=('denacc',))
                kb.op('dve', lambda e: e.tensor_tensor(out=QT[:, c0:c0 + CH], in0=numacc[:, c0:c0 + CH],
                                                       in1=denacc[:, c0:c0 + CH], op=ALU.mult),
                      r=('numacc', 'denacc'), w=('QT',))
            kb.dma('sp', odaT[h, :, :], QT[:], r=('QT',), w=('odaT',), acc=True)


def phase_b0(nc, kb, S, abT, dn_a_log, dn_dt_bias, gam, tokS_d, glb_d):
    C = P
    nch = S // C
    SEG = min(S, 2048)
    nchs = SEG // C
    with ExitStack() as es:
        identf, ident = make_identities(nc, kb, es)
        A = sb(nc, es, "A", [4, SEG], F32)
        B = sb(nc, es, "B", [4, SEG], F32)
        T1 = sb(nc, es, "T1", [4, SEG], F32)
        GF = sb(nc, es, "GF", [4, SEG], F32)
        MK = sb(nc, es, "MK", [4, SEG], F32)
        TR = sb(nc, es, "TR", [40, SEG], F32)
        par = [sb(nc, es, f"par{d}", [4, 4], F32) for d in range(2)]
        GL = sb(nc, es, "GL", [4, nchs], F32)
        sel = [sb(nc, es, f"sel{h}", [4, P], F32) for h in range(4)]
        tok = sb(nc, es, "tok", [P, nch, 40], F32)
        glb = sb(nc, es, "glb", [P, 8, nch], F32)
        pX = [ps(nc, es, f"pX{i}", [P, 512], F32) for i in range(2)]

        for h in range(4):
            kb.op('pool', lambda e: e.memset(sel[h][:], 0.0), w=(('sel', h),))
            kb.op('pool', lambda e: e.affine_select(out=sel[h][:], in_=sel[h][:], pattern=[[0, P]],
                                                    compare_op=ALU.not_equal, fill=1.0, base=-h,
                                                    channel_multiplier=1), r=(('sel', h),), w=(('sel', h),))
        kb.op('pool', lambda e: e.memset(MK[:], 1.0), w=('MK',))
        kb.op('pool', lambda e: e.memset(MK[:, 0:SEG:C], 0.0), w=('MK',))
        for d in range(2):
            kb.dma('sp', par[d][:, 0:1], dn_dt_bias[d:d + 1, :].rearrange("o h -> h o"), r=('dtb',), w=(('par', d),))
            kb.dma('sp', par[d][:, 1:2], dn_a_log[d:d + 1, :].rearrange("o h -> h o"), r=('alog',), w=(('par', d),),
                   acc=True)
            kb.op('act', lambda e: e.activation(out=par[d][:, 2:3], in_=par[d][:, 1:2], func=AF.Exp),
                  r=(('par', d),), w=(('par', d),))
            kb.op('dve', lambda e: e.tensor_scalar(out=par[d][:, 2:3], in0=par[d][:, 2:3], scalar1=-1.0, scalar2=None,
                                                   op0=ALU.mult), r=(('par', d),), w=(('par', d),))
        A3 = A[:, :].rearrange("p (c t) -> p c t", t=C)
        T13 = T1[:, :].rearrange("p (c t) -> p c t", t=C)
        GF3 = GF[:, :].rearrange("p (c t) -> p c t", t=C)
        for s0 in range(0, S, SEG):
            cb = s0 // C
            for d in range(2):
                pk = ('par', d)
                kb.dma('sp', A[:], abT[d * 4:(d + 1) * 4, s0:s0 + SEG], r=('abT',), w=('A',))
                kb.dma('sp', B[:], abT[8 + d * 4:8 + (d + 1) * 4, s0:s0 + SEG], r=('abT',), w=('B',))
                kb.op('dve', lambda e: e.tensor_scalar(out=A[:], in0=A[:], scalar1=par[d][:, 0:1], scalar2=None,
                                                       op0=ALU.add), r=('A', pk), w=('A',))
                kb.op('act', lambda e: e.activation(out=T1[:], in_=A[:], func=AF.Abs), r=('A',), w=('T1',))
                kb.op('act', lambda e: e.activation(out=T1[:], in_=T1[:], func=AF.Exp, scale=-1.0), r=('T1',),
                      w=('T1',))
                kb.op('act', lambda e: e.activation(out=T1[:], in_=T1[:], func=AF.Ln, bias=1.0), r=('T1',), w=('T1',))
                kb.op('dve', lambda e: e.scalar_tensor_tensor(out=A[:], in0=A[:], scalar=0.0, in1=T1[:], op0=ALU.max,
                                                              op1=ALU.add), r=('A', 'T1'), w=('A',))
                kb.op('dve', lambda e: e.tensor_scalar(out=A[:], in0=A[:], scalar1=par[d][:, 2:3], scalar2=None,
                                                       op0=ALU.mult), r=('A', pk), w=('A',))
                kb.op('dve', lambda e: e.tensor_tensor_scan(out=GF[:], data0=MK[:], data1=A[:], initial=0.0,
                                                            op0=ALU.mult, op1=ALU.add), r=('MK', 'A'), w=('GF',))
                tot = GF3[:, :, C - 1:C].to_broadcast([4, nchs, C])
                if d == 1:
                    kb.op('dve', lambda e: e.tensor_tensor(out=A[:], in0=A[:], in1=GF[:], op=ALU.subtract),
                          r=('A', 'GF'), w=('A',))
                    kb.op('dve', lambda e: e.tensor_tensor(out=A3, in0=A3, in1=tot, op=ALU.add), r=('A', 'GF'),
                          w=('A',))
                else:
                    kb.op('dve', lambda e: e.tensor_copy(out=A[:], in_=GF[:]), r=('GF',), w=('A',))
                kb.dma('sp', gam[d, :, s0:s0 + SEG], A[:], r=('A',), w=('gam',), acc=True)
                kb.op('act', lambda e: e.activation(out=B[:], in_=B[:], func=AF.Sigmoid), r=('B',), w=('B',))
                kb.dma('sp', TR[0 + d * 4:0 + d * 4 + 4, :], B[:], r=('B',), w=('TR',), acc=True, sk=('st', 'B'))
                kb.op('dve', lambda e: e.tensor_scalar(out=T1[:], in0=B[:], scalar1=-1.0, scalar2=None, op0=ALU.mult),
                      r=('B',), w=('T1',))
                kb.dma('sp', TR[24 + d * 4:24 + d * 4 + 4, :], T1[:], r=('T1',), w=('TR',), acc=True, sk=('st', 'T1'))
                kb.op('act', lambda e: e.activation(out=B[:], in_=A[:], func=AF.Exp), r=('A',), w=('B',))
                kb.dma('sp', TR[8 + d * 4:8 + d * 4 + 4, :], B[:], r=('B',), w=('TR',), acc=True, sk=('st', 'B'))
                kb.op('dve', lambda e: e.tensor_tensor(out=T13, in0=tot, in1=A3, op=ALU.subtract), r=('A', 'GF'),
                      w=('T1',))
                kb.op('act', lambda e: e.activation(out=T1[:], in_=T1[:], func=AF.Exp), r=('T1',), w=('T1',))
                kb.dma('sp', TR[16 + d * 4:16 + d * 4 + 4, :], T1[:], r=('T1',), w=('TR',), acc=True, sk=('st', 'T1'))
                kb.op('dve', lambda e: e.tensor_scalar(out=T1[:], in0=A[:], scalar1=-1.0, scalar2=None, op0=ALU.mult),
                      r=('A',), w=('T1',))
                kb.dma('sp', TR[32 + d * 4:32 + d * 4 + 4, :], T1[:], r=('T1',), w=('TR',), acc=True, sk=('st', 'T1'))
                kb.op('act', lambda e: e.activation(out=GL[:], in_=GF3[:, :, C - 1], func=AF.Exp), r=('GF',),
                      w=('GL',))
                for h in range(4):
                    kb.op('pe', lambda e: e.matmul(pX[0][:, 0:nchs], lhsT=sel[h][:], rhs=GL[:], start=True, stop=True),
                          r=(('sel', h), 'GL'), w=(('pX', 0),))
                    kb.op('dve', lambda e: e.tensor_copy(out=glb[:, d * 4 + h, cb:cb + nchs], in_=pX[0][:, 0:nchs]),
                          r=(('pX', 0),), w=('glb',))
            for c in range(nchs):
                s = c % 2
                kb.op('pe', lambda e: e.transpose(out=pX[s][:, 0:40], in_=TR[:, c * C:(c + 1) * C],
                                                  identity=identf[0:40, 0:40]),
                      r=('TR', 'identf'), w=(('pX', s),))
                kb.op('act', lambda e: e.activation(out=tok[:, cb + c, :], in_=pX[s][:, 0:40], func=AF.Copy),
                      r=(('pX', s),), w=('tok',))
        kb.dma('sp', tokS_d[:, :, :], tok[:], r=('tok',), w=('tokS_d',))
        kb.dma('sp', glb_d[:, :, :], glb[:], r=('glb',), w=('glb_d',))


def phase_b1(nc, kb, S, projT, dn_conv, dnq, dnk, dnkt, dnvt, bg=None):
    C = P
    nch = S // C
    HS = min(S, 2048)

    def bg_step(k):
        if bg is None:
            return
        for _ in range(k):
            try:
                next(bg)
            except StopIteration:
                return

    with ExitStack() as es:
        identf, ident = make_identities(nc, kb, es)
        onesb = sb(nc, es, "onesb", [P, P], BF16)
        cw = sb(nc, es, "cw", [P, 12, 4], F32)
        xin = [sb(nc, es, f"xin{i}", [P, S + 3], BF16) for i in range(2)]
        acc = sb(nc, es, "acc", [P, HS], F32)
        sq = sb(nc, es, "sq", [P, HS], BF16)
        rn = sb(nc, es, "rn", [P, 512], F32)
        yT = [sb(nc, es, f"yT{i}", [P, S], BF16) for i in range(2)]
        ytok = [sb(nc, es, f"ytok{i}", [P, nch, P], BF16) for i in range(2)]
        pX = [ps(nc, es, f"pX{i}", [P, 512], F32) for i in range(2)]
        pV = [ps(nc, es, f"pV{i}", [P, P], BF16) for i in range(2)]
        kb.op('pool', lambda e: e.memset(onesb[:], 1.0), w=('onesb',))
        for t in range(12):
            kb.dma('sp', cw[:, t, :], dn_conv[:, t * P:(t + 1) * P].rearrange("k c -> c k"), r=('dn_conv',),
                   w=('cw',), acc=True, allow_slow_non_contiguous=True)
        n = 0
        for kind in range(3):
            for h in range(4):
                t = kind * 4 + h
                s = n % 2
                n += 1
                kb.op('pool', lambda e: e.memset(xin[s][:, 0:2], 0.0), w=(('xin', s),))
                kb.op('pool', lambda e: e.memset(xin[s][:, S + 2:S + 3], 0.0), w=(('xin', s),))
                kb.dma('sp', xin[s][:, 2:S + 2], projT[t, :, :], r=('projT',), w=(('xin', s),), acc=True)
                for c0 in range(0, S, HS):
                    kb.op('dve', lambda e: e.tensor_scalar(out=acc[:], in0=xin[s][:, c0:c0 + HS],
                                                           scalar1=cw[:, t, 0:1], scalar2=None, op0=ALU.mult),
                          r=(('xin', s), 'cw'), w=('acc',))
                    for k in range(1, 4):
                        kb.op('dve', lambda e: e.scalar_tensor_tensor(out=acc[:], in0=xin[s][:, c0 + k:c0 + k + HS],
                                                                      scalar=cw[:, t, k:k + 1], in1=acc[:],
                                                                      op0=ALU.mult, op1=ALU.add),
                              r=(('xin', s), 'cw', 'acc'), w=('acc',))
                    if kind == 2:
                        kb.op('act', lambda e: e.activation(out=yT[s][:, c0:c0 + HS], in_=acc[:], func=AF.Silu),
                              r=('acc',), w=(('yT', s),))
                        bg_step(2)
                        continue
                    kb.op('act', lambda e: e.activation(out=acc[:], in_=acc[:], func=AF.Silu), r=('acc',), w=('acc',))
                    kb.op('act', lambda e: e.activation(out=sq[:], in_=acc[:], func=AF.Square), r=('acc',), w=('sq',))
                    for j in range(0, HS, 512):
                        b = (j // 512) % 2
                        kb.op('pe', lambda e: e.matmul(pX[b][:], lhsT=onesb[:], rhs=sq[:, j:j + 512], start=True,
                                                       stop=True), r=('onesb', 'sq'), w=(('pX', b),))
                        kb.op('act', lambda e: e.activation(out=rn[:], in_=pX[b][:], func=AF.Ln, bias=EPS),
                              r=(('pX', b),), w=('rn',))
                        kb.op('act', lambda e: e.activation(out=rn[:], in_=rn[:], func=AF.Exp, scale=-0.5), r=('rn',),
                              w=('rn',))
                        bg_step(1)
                        sc = (128 ** -0.5) if kind == 0 else 1.0
                        kb.op('dve', lambda e: e.scalar_tensor_tensor(out=yT[s][:, c0 + j:c0 + j + 512],
                                                                      in0=acc[:, j:j + 512], scalar=sc, in1=rn[:],
                                                                      op0=ALU.mult, op1=ALU.mult),
                              r=('acc', 'rn'), w=(('yT', s),))
                if kind == 0:
                    kb.dma('pool', dnq[h, :, :], yT[s][:], r=(('yT', s),), w=('dnq',), acc=True)
                    continue
                if kind == 1:
                    kb.dma('pool', dnk[h, :, :], yT[s][:], r=(('yT', s),), w=('dnk',), acc=True)
                for c in range(nch):
                    b = c % 2
                    kb.op('pe', lambda e: e.transpose(out=pV[b][:], in_=yT[s][:, c * C:(c + 1) * C], identity=ident[:]),
                          r=(('yT', s), 'ident'), w=(('pV', b),))
                    if c % 2 == 0:
                        kb.op('act', lambda e: e.activation(out=ytok[s][:, c, :], in_=pV[b][:], func=AF.Copy),
                              r=(('pV', b),), w=(('ytok', s),))
                    else:
                        kb.op('dve', lambda e: e.tensor_copy(out=ytok[s][:, c, :], in_=pV[b][:]),
                              r=(('pV', b),), w=(('ytok', s),))
                dst = dnkt if kind == 1 else dnvt
                kb.dma('pool', dst[h, :, :, :], ytok[s][:], r=(('ytok', s),), w=('dnkt' if kind == 1 else 'dnvt',),
                       acc=True)


def phase_b2(nc, kb, S, projT, dnq, dnk, dnkt, dnvt, gam, tokS_d, glb_d, dn_out_norm, odnT):
    C = P
    nch = S // C
    with ExitStack() as es:
        identf, ident = make_identities(nc, kb, es)
        onesb = sb(nc, es, "onesb", [P, P], BF16)
        MN = [sb(nc, es, f"MN{d}", [P, P], F32) for d in range(2)]
        SMf = sb(nc, es, "SMf", [P, P], F32)
        SM = [sb(nc, es, f"SM{d}", [P, P], BF16) for d in range(2)]
        sel = [sb(nc, es, f"sel{d}", [2, P], F32) for d in range(2)]
        nsel = [sb(nc, es, f"nsel{d}", [2, P], F32) for d in range(2)]
        tokS = sb(nc, es, "tokS", [P, nch, 40], F32)
        glb = sb(nc, es, "glb", [P, 8, nch], F32)
        onorm = sb(nc, es, "onorm", [P, 1], F32)
        qT = sb(nc, es, "qT", [P, S], BF16)
        kT = sb(nc, es, "kT", [P, S], BF16)
        ktok = sb(nc, es, "ktok", [P, nch, P], BF16)
        vtok = sb(nc, es, "vtok", [P, nch, P], BF16)
        G2 = sb(nc, es, "G2", [2, S], F32)
        oacc = sb(nc, es, "oacc", [P, S], F32)
        rn = sb(nc, es, "rn", [P, 512], F32)
        Sf = [sb(nc, es, f"Sf{d}", [P, P], F32) for d in range(2)]
        Sb = [[sb(nc, es, f"Sb{d}{i}", [P, P], BF16) for i in range(2)] for d in range(2)]

        def two(name, dt):
            return [sb(nc, es, f"{name}{i}", [P, P], dt) for i in range(2)]
        DT = two("DT", F32)
        intraT = two("intraT", BF16)
        Mf = two("Mf", F32)
        Pm = [two("Pa", BF16), two("Pb", BF16)]
        Qm = [two("Qa", BF16), two("Qb", BF16)]
        Rm = [two("Ra", BF16), two("Rb", BF16)]
        ub = two("ub", F32)
        kg = two("kg", BF16)
        kd = two("kd", BF16)
        wT = two("wT", BF16)
        egb = two("egb", F32)
        qgT = two("qgT", BF16)
        vnew = two("vnew", BF16)

        banks = [ps(nc, es, f"bk{i}", [P, 512], F32) for i in range(7)]
        bankb = ps(nc, es, "bkb", [P, 1024], BF16)
        for i in range(8):
            kb.excl.add(('bank', i))

        def sub(bank, col):
            return banks[bank][:, col * P:(col + 1) * P]
        PS = []
        for d in range(2):
            a, b, c = 3 * d, 3 * d + 1, 3 * d + 2
            PS.append({
                'pE': (sub(a, 0), ('bank', a)), 'pB': (sub(a, 1), ('bank', a)), 'pG': (sub(a, 2), ('bank', a)),
                'pQK': (sub(a, 3), ('bank', a)),
                'pA0': (sub(b, 0), ('bank', b)), 'pR': (sub(b, 1), ('bank', b)), 'pU': (sub(b, 2), ('bank', b)),
                'pW': (sub(b, 3), ('bank', b)),
                'pA1': (sub(c, 0), ('bank', c)), 'pWS': (sub(c, 1), ('bank', c)), 'pO': (sub(c, 2), ('bank', c)),
                'pS2': (sub(c, 3), ('bank', c)),
                'pTb': (bankb[:, d * P:(d + 1) * P], ('bank', 7)),
            })
        pX = banks[6]

        kb.op('pool', lambda e: e.memset(onesb[:], 1.0), w=('onesb',))
        for d in range(2):
            kb.op('pool', lambda e: e.memset(sel[d][:], 0.0), w=(('sel', d),))
            kb.op('pool', lambda e: e.affine_select(out=sel[d][:], in_=sel[d][:], pattern=[[0, P]],
                                                    compare_op=ALU.not_equal, fill=1.0, base=-d,
                                                    channel_multiplier=1), r=(('sel', d),), w=(('sel', d),))
            kb.op('pool', lambda e: e.memset(nsel[d][:], 0.0), w=(('nsel', d),))
            kb.op('pool', lambda e: e.affine_select(out=nsel[d][:], in_=nsel[d][:], pattern=[[0, P]],
                                                    compare_op=ALU.not_equal, fill=-1.0, base=-d,
                                                    channel_multiplier=1), r=(('nsel', d),), w=(('nsel', d),))
        for d in range(2):
            cm, st = (-1, 1) if d == 0 else (1, -1)
            kb.op('pool', lambda e: e.memset(MN[d][:], 0.0), w=(('MN', d),))
            kb.op('pool', lambda e: e.affine_select(out=MN[d][:], in_=MN[d][:], pattern=[[st, P]],
                                                    compare_op=ALU.is_ge, fill=-1e30, base=0, channel_multiplier=cm),
                  r=(('MN', d),), w=(('MN', d),))
            kb.op('pool', lambda e: e.memset(SMf[:], 1.0), w=('SMf',))
            kb.op('pool', lambda e: e.affine_select(out=SMf[:], in_=SMf[:], pattern=[[st, P]],
                                                    compare_op=ALU.is_gt, fill=0.0, base=0, channel_multiplier=cm),
                  r=('SMf',), w=('SMf',))
            kb.op('dve', lambda e: e.tensor_copy(out=SM[d][:], in_=SMf[:]), r=('SMf',), w=(('SM', d),))
            kb.op('pool', lambda e: e.memset(MN[d][:], 1.0), r=(('MN', d),), w=(('MN', d),))
            kb.op('pool', lambda e: e.affine_select(out=MN[d][:], in_=MN[d][:], pattern=[[st, P]],
                                                    compare_op=ALU.is_ge, fill=0.0, base=0, channel_multiplier=cm),
                  r=(('MN', d),), w=(('MN', d),))
        kb.dma('sp', tokS[:], tokS_d[:, :, :], r=('tokS_d',), w=('tokS',))
        kb.dma('sp', glb[:], glb_d[:, :, :], r=('glb_d',), w=('glb',))
        kb.dma('sp', onorm[:], dn_out_norm[0:1, :].rearrange("o c -> c o"), r=('dn_out_norm',), w=('onorm',))

        def chunk(h, d, c, step):
            s = d
            dh = d * 4 + h
            cs = slice(c * C, (c + 1) * C)
            beta = tokS[:, c, 0 + dh:0 + dh + 1]
            egc = tokS[:, c, 8 + dh:8 + dh + 1]
            ekd = tokS[:, c, 16 + dh:16 + dh + 1]
            nbeta = tokS[:, c, 24 + dh:24 + dh + 1]
            pp = PS[d]
            K = lambda n: pp[n][1] if n in pp else (n, s)
            T = lambda n: pp[n][0]
            negG = tokS[:, c, 32 + dh:32 + dh + 1]
            kb.op('pe', lambda e: e.matmul(T('pB'), lhsT=sel[d][:], rhs=G2[:, cs], start=True, stop=True),
                  r=('G2', ('sel', d)), w=(K('pB'),))
            yield
            kb.op('dve', lambda e: e.tensor_scalar(out=Mf[s][:], in0=T('pB'), scalar1=negG, scalar2=0.0,
                                                   op0=ALU.add, op1=ALU.min), r=(K('pB'), 'tokS'), w=(K('Mf'),))
            kb.op('act', lambda e: e.activation(out=egb[s][:], in_=T('pB'), func=AF.Exp), r=(K('pB'),),
                  w=(K('egb'),))
            yield
            kb.op('act', lambda e: e.activation(out=DT[s][:], in_=Mf[s][:], func=AF.Exp), r=(K('Mf'),),
                  w=(K('DT'),))
            yield
            kb.op('pool', lambda e: e.tensor_tensor(out=DT[s][:], in0=DT[s][:], in1=MN[d][:], op=ALU.mult),
                  r=(K('DT'), ('MN', d)), w=(K('DT'),))
            yield
            kb.op('pe', lambda e: e.matmul(T('pG'), lhsT=kT[:, cs], rhs=kT[:, cs], start=True, stop=True),
                  r=('kT',), w=(K('pG'),))
            kb.op('pe', lambda e: e.matmul(T('pQK'), lhsT=kT[:, cs], rhs=qT[:, cs], start=True, stop=True),
                  r=('kT', 'qT'), w=(K('pQK'),))
            yield
            kb.op('dve', lambda e: e.scalar_tensor_tensor(out=Mf[s][:], in0=T('pG'), scalar=beta, in1=DT[s][:],
                                                          op0=ALU.mult, op1=ALU.mult),
                  r=(K('pG'), K('DT'), 'tokS'), w=(K('Mf'),))
            kb.op('dve', lambda e: e.tensor_tensor(out=intraT[s][:], in0=T('pQK'), in1=DT[s][:], op=ALU.mult),
                  r=(K('pQK'), K('DT')), w=(K('intraT'),))
            yield
            P0, Q0, R0 = Pm[0][s], Qm[0][s], Rm[0][s]
            kb.op('pool', lambda e: e.tensor_tensor(out=P0[:], in0=Mf[s][:], in1=SM[d][:], op=ALU.mult),
                  r=(K('Mf'), ('SM', d)), w=(K('P0'),))
            kb.op('pool', lambda e: e.tensor_tensor(out=R0[:], in0=ident[:], in1=P0[:], op=ALU.subtract),
                  r=('ident', K('P0')), w=(K('R0'),))
            yield
            kb.op('pe', lambda e: e.transpose(out=T('pTb'), in_=P0[:], identity=ident[:]),
                  r=(K('P0'), 'ident'), w=(K('pTb'),))
            yield
            kb.op('act', lambda e: e.activation(out=Q0[:], in_=T('pTb'), func=AF.Copy), r=(K('pTb'),),
                  w=(K('Q0'),))
            yield
            for k in range(1, 7):
                a, b = (k - 1) % 2, k % 2
                Pp, Qp, Rp = Pm[a][s], Qm[a][s], Rm[a][s]
                Pn, Qn, Rn = Pm[b][s], Qm[b][s], Rm[b][s]
                pa, pb_ = f'P{a}', f'P{b}'
                qa, qb_ = f'Q{a}', f'Q{b}'
                ra, rb_ = f'R{a}', f'R{b}'
                kb.op('pe', lambda e: e.matmul(T('pA0'), lhsT=Pp[:], rhs=Qp[:], start=True, stop=True),
                      r=(K(pa), K(qa)), w=(K('pA0'),))
                if k < 6:
                    kb.op('pe', lambda e: e.matmul(T('pA1'), lhsT=Qp[:], rhs=Pp[:], start=True, stop=True),
                          r=(K(pa), K(qa)), w=(K('pA1'),))
                yield
                kb.op('act', lambda e: e.activation(out=Qn[:], in_=T('pA0'), func=AF.Copy), r=(K('pA0'),),
                      w=(K(qb_),))
                if k < 6:
                    kb.op('dve', lambda e: e.tensor_copy(out=Pn[:], in_=T('pA1')), r=(K('pA1'),), w=(K(pb_),))
                yield
                kb.op('pe', lambda e: e.matmul(T('pR'), lhsT=Qn[:], rhs=Rp[:], start=True, stop=True),
                      r=(K(qb_), K(ra)), w=(K('pR'),))
                yield
                kb.op('dve', lambda e: e.tensor_tensor(out=Rn[:], in0=Rp[:], in1=T('pR'), op=ALU.add),
                      r=(K(ra), K('pR')), w=(K(rb_),))
                yield
            TT = Rm[0][s]
            kT_ = K('R0')
            kb.op('pool', lambda e: e.tensor_scalar(out=kg[s][:], in0=ktok[:, c, :], scalar1=egc, scalar2=None,
                                                    op0=ALU.mult), r=('ktok', 'tokS'), w=(K('kg'),))
            kb.op('pool', lambda e: e.tensor_scalar(out=kd[s][:], in0=ktok[:, c, :], scalar1=ekd, scalar2=None,
                                                    op0=ALU.mult), r=('ktok', 'tokS'), w=(K('kd'),))
            yield
            kb.op('pe', lambda e: e.matmul(T('pU'), lhsT=TT[:], rhs=vtok[:, c, :], start=True, stop=True),
                  r=(kT_, 'vtok'), w=(K('pU'),))
            kb.op('pe', lambda e: e.matmul(T('pW'), lhsT=kg[s][:], rhs=TT[:], start=True, stop=True),
                  r=(K('kg'), kT_), w=(K('pW'),))
            yield
            kb.op('act', lambda e: e.activation(out=ub[s][:], in_=T('pU'), func=AF.Copy, scale=beta),
                  r=(K('pU'), 'tokS'), w=(K('ub'),))
            kb.op('act', lambda e: e.activation(out=wT[s][:], in_=T('pW'), func=AF.Copy), r=(K('pW'),),
                  w=(K('wT'),))
            kb.op('dve', lambda e: e.tensor_tensor(out=qgT[s][:], in0=qT[:, cs], in1=egb[s][:], op=ALU.mult),
                  r=('qT', K('egb')), w=(K('qgT'),))
            yield
            so, sn = step % 2, (step + 1) % 2
            kb.op('pe', lambda e: e.matmul(T('pWS'), lhsT=wT[s][:], rhs=Sb[d][so][:], start=True, stop=True),
                  r=(K('wT'), ('Sb', d, so)), w=(K('pWS'),))
            yield
            kb.op('dve', lambda e: e.scalar_tensor_tensor(out=vnew[s][:], in0=T('pWS'), scalar=nbeta,
                                                          in1=ub[s][:], op0=ALU.mult, op1=ALU.add),
                  r=(K('pWS'), K('ub'), 'tokS'), w=(K('vnew'),))
            yield
            kb.op('pe', lambda e: e.matmul(T('pO'), lhsT=Sb[d][so][:], rhs=qgT[s][:], start=True, stop=False),
                  r=(('Sb', d, so), K('qgT')), w=(K('pO'),))
            kb.op('pe', lambda e: e.matmul(T('pO'), lhsT=vnew[s][:], rhs=intraT[s][:], start=False, stop=True),
                  r=(K('vnew'), K('intraT')), w=(K('pO'),))
            kb.op('pe', lambda e: e.matmul(T('pS2'), lhsT=kd[s][:], rhs=vnew[s][:], start=True, stop=True),
                  r=(K('kd'), K('vnew')), w=(K('pS2'),))
            yield
            first = (c < nch // 2) if d == 0 else (c >= nch // 2)
            if first:
                kb.op('act', lambda e: e.activation(out=oacc[:, cs], in_=T('pO'), func=AF.Copy),
                      r=(K('pO'),), w=(('oacc', c),))
            else:
                kb.op('dve', lambda e: e.tensor_tensor(out=oacc[:, cs], in0=oacc[:, cs], in1=T('pO'),
                                                       op=ALU.add), r=(K('pO'), ('oacc', c)), w=(('oacc', c),))
            kb.op('dve', lambda e: e.scalar_tensor_tensor(out=Sf[d][:], in0=Sf[d][:], scalar=glb[:, dh, c:c + 1],
                                                          in1=T('pS2'), op0=ALU.mult, op1=ALU.add),
                  r=(('Sf', d), 'glb', K('pS2')), w=(('Sf', d),))
            yield
            kb.op('act', lambda e: e.activation(out=Sb[d][sn][:], in_=Sf[d][:], func=AF.Copy), r=(('Sf', d),),
                  w=(('Sb', d, sn),))
            yield

        for h in range(4):
            kb.dma('sp', qT[:], dnq[h, :, :], r=('dnq',), w=('qT',))
            kb.dma('sp', kT[:], dnk[h, :, :], r=('dnk',), w=('kT',))
            kb.dma('sp', ktok[:], dnkt[h, :, :, :], r=('dnkt',), w=('ktok',))
            kb.dma('sp', vtok[:], dnvt[h, :, :, :], r=('dnvt',), w=('vtok',))
            kb.dma('sp', G2[0:1, :], gam[0, h:h + 1, :], r=('gam',), w=('G2',))
            kb.dma('sp', G2[1:2, :], gam[1, h:h + 1, :], r=('gam',), w=('G2',), acc=True)
            for d in range(2):
                kb.op('pool', lambda e: e.memset(Sf[d][:], 0.0), w=(('Sf', d),))
                kb.op('pool', lambda e: e.memset(Sb[d][0][:], 0.0), w=(('Sb', d, 0),))
            for step in range(nch):
                g0 = chunk(h, 0, step, step)
                g1 = chunk(h, 1, nch - 1 - step, step)
                done0 = done1 = False
                while not (done0 and done1):
                    if not done0:
                        try:
                            next(g0)
                        except StopIteration:
                            done0 = True
                    if not done1:
                        try:
                            next(g1)
                        except StopIteration:
                            done1 = True
            allo = tuple(('oacc', c) for c in range(nch))
            kb.dma('sp', kT[:], projT[12 + h, :, :], r=('projT',), w=('kT',))
            for j in range(0, S, 512):
                js = slice(j, j + 512)
                ok_ = tuple(('oacc', c) for c in range(j // C, (j + 512) // C))
                kb.op('act', lambda e: e.activation(out=qT[:, js], in_=oacc[:, js], func=AF.Square), r=ok_,
                      w=('qT',))
                kb.op('pe', lambda e: e.matmul(pX[:], lhsT=onesb[:], rhs=qT[:, js], start=True, stop=True),
                      r=('onesb', 'qT'), w=(('bank', 6),))
                kb.op('act', lambda e: e.activation(out=rn[:], in_=pX[:], func=AF.Ln, scale=1.0 / P, bias=EPS),
                      r=(('bank', 6),), w=('rn',))
                kb.op('act', lambda e: e.activation(out=rn[:], in_=rn[:], func=AF.Exp, scale=-0.5), r=('rn',),
                      w=('rn',))
                kb.op('dve', lambda e: e.scalar_tensor_tensor(out=rn[:], in0=rn[:], scalar=onorm[:, 0:1],
                                                              in1=oacc[:, js], op0=ALU.mult, op1=ALU.mult),
                      r=('rn', 'onorm') + ok_, w=('rn',))
                kb.op('dve', lambda e: e.tensor_tensor(out=qT[:, js], in0=rn[:], in1=kT[:, js], op=ALU.mult),
                      r=('rn', 'kT'), w=('qT',))
            kb.dma('sp', odnT[h, :, :], qT[:], r=('qT',), w=('odnT',), acc=True)


def phase_d(nc, kb, S, x, projT, odnT, odaT, w_bdn, w_bda, w_out, norm_ffn, peer_wq, sub_keys, hbuf, xn2d, scores_d):
    ST = 512
    with ExitStack() as es:
        identf, ident = make_identities(nc, kb, es)
        Wbdn = sb(nc, es, "Wbdn", [P, 4, D], BF16)
        Wbda = sb(nc, es, "Wbda", [P, 4, D], BF16)
        Wout = sb(nc, es, "Wout", [P, KC, D], BF16)
        Wq = sb(nc, es, "Wq", [P, KC, 2048], BF16)
        skT = sb(nc, es, "skT", [P, 16, P], BF16)
        gffn = sb(nc, es, "gffn", [P, D], F32)
        stage = [sb(nc, es, f"stage{i}", [P, D], F32) for i in range(2)]
        odn = sb(nc, es, "odn", [P, 4, ST], BF16)
        oda = sb(nc, es, "oda", [P, 4, ST], BF16)
        gdn = sb(nc, es, "gdn", [P, 8, ST], BF16)
        gda = sb(nc, es, "gda", [P, 8, ST], BF16)
        mergedT = sb(nc, es, "mergedT", [P, 8, ST], BF16)
        t1 = sb(nc, es, "t1", [P, ST], F32)
        t2 = sb(nc, es, "t2", [P, ST], F32)
        xt = sb(nc, es, "xt", [P, D], F32)
        ht = [sb(nc, es, f"ht{i}", [P, D], F32) for i in range(2)]
        junk = sb(nc, es, "junk", [P, D], BF16)
        ss = sb(nc, es, "ss", [P, 2], F32)
        xn2 = [sb(nc, es, f"xn2{i}", [P, D], BF16) for i in range(2)]
        xn2T = sb(nc, es, "xn2T", [P, KC, ST], BF16)
        qhT = sb(nc, es, "qhT", [P, 16, ST], BF16)
        sc = [sb(nc, es, f"sc{i}", [P, 2048], F32) for i in range(2)]
        pM = [ps(nc, es, f"pM{i}", [P, ST], F32) for i in range(2)]
        pH = [ps(nc, es, f"pH{i}", [P, ST], F32) for i in range(2)]
        pT = ps(nc, es, "pT", [P, KC, P], BF16)
        pQ = ps(nc, es, "pQ", [P, ST], F32)
        pSc = [ps(nc, es, f"pSc{i}", [P, ST], F32) for i in range(2)]

        kb.dma('sp', gffn[:], norm_ffn[0:1, :].partition_broadcast(P), r=('norm_ffn',), w=('gffn',))
        n = [0]

        def loadw(dst, src, nk, ncols, key):
            for kc in range(nk):
                for c0 in range(0, ncols, D):
                    s_ = n[0] % 2
                    n[0] += 1
                    kb.dma('sp', stage[s_][:], src[kc * P:(kc + 1) * P, c0:c0 + D], r=(key + '_d',), w=(('stage', s_),))
                    if s_ == 0:
                        kb.op('act', lambda e: e.activation(out=dst[:, kc, c0:c0 + D], in_=stage[s_][:], func=AF.Copy),
                              r=(('stage', s_),), w=(key,))
                    else:
                        kb.op('dve', lambda e: e.tensor_copy(out=dst[:, kc, c0:c0 + D], in_=stage[s_][:]),
                              r=(('stage', s_),), w=(key,))
        loadw(Wbdn, w_bdn, 4, D, 'Wbdn')
        loadw(Wbda, w_bda, 4, D, 'Wbda')
        loadw(Wout, w_out, KC, D, 'Wout')
        loadw(Wq, peer_wq, KC, 2048, 'Wq')
        for hc in range(16):
            s_ = n[0] % 2
            n[0] += 1
            kb.dma('sp', stage[s_][:, 0:P], sub_keys[hc, :, :], r=('sub_keys',), w=(('stage', s_),))
            kb.op('pe', lambda e: e.transpose(out=pQ[:, 0:P], in_=stage[s_][:, 0:P], identity=identf[:]),
                  r=(('stage', s_), 'identf'), w=('pQ',))
            kb.op('dve', lambda e: e.tensor_copy(out=skT[:, hc, :], in_=pQ[:, 0:P]), r=('pQ',), w=('skT',))

        ti = 0
        for st in range(S // ST):
            tk = slice(st * ST, (st + 1) * ST)
            kb.dma('sp', odn[:], odnT[:, :, tk].rearrange("c p t -> p c t"), r=('odnT',), w=('odn',))
            kb.dma('sp', oda[:], odaT[:, :, tk].rearrange("c p t -> p c t"), r=('odaT',), w=('oda',))
            kb.dma('sp', gdn[:], projT[52:60, :, tk].rearrange("c p t -> p c t"), r=('projT',), w=('gdn',))
            kb.dma('sp', gda[:], projT[60:68, :, tk].rearrange("c p t -> p c t"), r=('projT',), w=('gda',))
            for m in range(8):
                for kc in range(4):
                    kb.op('pe', lambda e: e.matmul(pM[0][:], lhsT=Wbdn[:, kc, m * P:(m + 1) * P], rhs=odn[:, kc, :],
                                                   start=(kc == 0), stop=(kc == 3)), r=('Wbdn', 'odn'), w=(('pM', 0),))
                for kc in range(4):
                    kb.op('pe', lambda e: e.matmul(pM[1][:], lhsT=Wbda[:, kc, m * P:(m + 1) * P], rhs=oda[:, kc, :],
                                                   start=(kc == 0), stop=(kc == 3)), r=('Wbda', 'oda'), w=(('pM', 1),))
                kb.op('dve', lambda e: e.tensor_tensor(out=t1[:], in0=pM[0][:], in1=gdn[:, m, :], op=ALU.mult),
                      r=(('pM', 0), 'gdn'), w=('t1',))
                kb.op('dve', lambda e: e.tensor_tensor(out=t2[:], in0=pM[1][:], in1=gda[:, m, :], op=ALU.mult),
                      r=(('pM', 1), 'gda'), w=('t2',))
                kb.op('pool', lambda e: e.tensor_tensor(out=mergedT[:, m, :], in0=t1[:], in1=t2[:], op=ALU.add),
                      r=('t1', 't2'), w=('mergedT',))
            for j in range(ST // P):
                s_ = ti % 2
                ti += 1
                t0 = st * ST + j * P
                kb.dma('sp', xt[:], x[t0:t0 + P, :], r=('x',), w=('xt',))
                for nh in range(2):
                    for mc in range(8):
                        kb.op('pe', lambda e: e.matmul(pH[nh][:], lhsT=mergedT[:, mc, j * P:(j + 1) * P],
                                                       rhs=Wout[:, mc, nh * ST:(nh + 1) * ST], start=(mc == 0),
                                                       stop=(mc == 7)), r=('mergedT', 'Wout'), w=(('pH', nh),))
                    kb.op('dve', lambda e: e.tensor_tensor(out=ht[s_][:, nh * ST:(nh + 1) * ST],
                                                           in0=xt[:, nh * ST:(nh + 1) * ST], in1=pH[nh][:], op=ALU.add),
                          r=('xt', ('pH', nh)), w=(('ht', s_),))
                kb.dma('pool', hbuf[t0:t0 + P, :], ht[s_][:], r=(('ht', s_),), w=('hbuf',), acc=True)
                kb.op('act', lambda e: e.activation(out=junk[:], in_=ht[s_][:], func=AF.Square, scale=D ** -0.5,
                                                    accum_out=ss[:, 0:1]), r=(('ht', s_),), w=('junk', 'ss'))
                kb.op('dve', lambda e: e.tensor_scalar(out=ss[:, 1:2], in0=ss[:, 0:1], scalar1=EPS, scalar2=None,
                                                       op0=ALU.add), r=('ss',), w=('ss',))
                kb.op('act', lambda e: e.activation(out=ss[:, 1:2], in_=ss[:, 1:2], func=AF.Sqrt), r=('ss',), w=('ss',))
                kb.op('dve', lambda e: e.reciprocal(out=ss[:, 1:2], in_=ss[:, 1:2]), r=('ss',), w=('ss',))
                kb.op('dve', lambda e: e.scalar_tensor_tensor(out=xn2[s_][:], in0=ht[s_][:], scalar=ss[:, 1:2],
                                                              in1=gffn[:], op0=ALU.mult, op1=ALU.mult),
                      r=(('ht', s_), 'ss', 'gffn'), w=(('xn2', s_),))
                kb.dma('pool', xn2d[t0:t0 + P, :], xn2[s_][:], r=(('xn2', s_),), w=('xn2d',), acc=True)
                for kc in range(KC):
                    kb.op('pe', lambda e: e.transpose(out=pT[:, kc, :], in_=xn2[s_][:, kc * P:(kc + 1) * P],
                                                      identity=ident[:]), r=(('xn2', s_), 'ident'), w=('pT',))
                kb.op('act', lambda e: e.activation(out=xn2T[:, :, j * P:(j + 1) * P], in_=pT[:], func=AF.Copy),
                      r=('pT',), w=('xn2T',))
            for hc in range(16):
                for kc in range(KC):
                    kb.op('pe', lambda e: e.matmul(pQ[:], lhsT=Wq[:, kc, hc * P:(hc + 1) * P], rhs=xn2T[:, kc, :],
                                                   start=(kc == 0), stop=(kc == KC - 1)), r=('Wq', 'xn2T'), w=('pQ',))
                if hc % 2 == 0:
                    kb.op('act', lambda e: e.activation(out=qhT[:, hc, :], in_=pQ[:], func=AF.Copy), r=('pQ',),
                          w=('qhT',))
                else:
                    kb.op('dve', lambda e: e.tensor_copy(out=qhT[:, hc, :], in_=pQ[:]), r=('pQ',), w=('qhT',))
            for j in range(ST // P):
                t0 = st * ST + j * P
                s_ = j % 2
                for bk in range(4):
                    b = bk % 2
                    for q in range(4):
                        hc = bk * 4 + q
                        kb.op('pe', lambda e: e.matmul(pSc[b][:, q * P:(q + 1) * P], lhsT=qhT[:, hc, j * P:(j + 1) * P],
                                                       rhs=skT[:, hc, :], start=True, stop=True),
                              r=('qhT', 'skT'), w=(('pSc', b),))
                    if bk % 2 == 0:
                        kb.op('act', lambda e: e.activation(out=sc[s_][:, bk * ST:(bk + 1) * ST], in_=pSc[b][:],
                                                            func=AF.Copy), r=(('pSc', b),), w=(('sc', s_),))
                    else:
                        kb.op('dve', lambda e: e.tensor_copy(out=sc[s_][:, bk * ST:(bk + 1) * ST], in_=pSc[b][:]),
                              r=(('pSc', b),), w=(('sc', s_),))
                kb.dma('pool', scores_d[t0:t0 + P, :], sc[s_][:], r=(('sc', s_),), w=('scores_d',), acc=True)


def phase_p_gen(nc, kb, es, peer_u, peer_v, uvb_d):
    NSL = 4
    RB = 2
    st_, ob_ = es
    n = 0
    for (src, dst, nm) in ((peer_u, uvb_d[:, 0:D], 'uvb_d'), (peer_v, uvb_d[:, D:2 * D], 'uvb_d')):
        for r0 in range(0, 16384, RB * P):
            s_ = n % NSL
            n += 1
            kb.dma('sp', st_[s_][:], src[r0:r0 + RB * P, :].rearrange("(a p) d -> p a d", p=P), r=('peer_tab',),
                   w=(('pst', s_),))
            if n % 2 == 0:
                kb.op('act', lambda e: e.activation(out=ob_[s_][:], in_=st_[s_][:], func=AF.Copy),
                      r=(('pst', s_),), w=(('pob', s_),))
            else:
                kb.op('pool', lambda e: e.tensor_copy(out=ob_[s_][:], in_=st_[s_][:]), r=(('pst', s_),),
                      w=(('pob', s_),))
            kb.dma('act', dst[r0:r0 + RB * P, :].rearrange("(a p) d -> p a d", p=P), ob_[s_][:],
                   r=(('pob', s_),), w=(nm,), acc=True)
            yield


def phase_e(nc, kb, S, hbuf, xn2d, scores_d, uvb_d, norm_final, y):
    NS = 4
    NB = 6
    GC = 0.7978845608028654
    nt = S // P
    with ExitStack() as es:
        identf, ident = make_identities(nc, kb, es)
        gfin = sb(nc, es, "gfin", [P, D], F32)
        act4 = [sb(nc, es, f"act4{i}", [P, NS], F32) for i in range(2)]
        g14 = [sb(nc, es, f"g14{i}", [P, NS], F32) for i in range(2)]
        g24 = [sb(nc, es, f"g24{i}", [P, NS], F32) for i in range(2)]
        c44 = [sb(nc, es, f"c44{i}", [P, NS], F32) for i in range(2)]
        dg = [sb(nc, es, f"dg{i}", [P, P], BF16) for i in range(4)]
        pO = [[ps(nc, es, f"pO{i}{j}", [P, 512], F32) for j in range(2)] for i in range(2)]
        sc = [sb(nc, es, f"sc{i}", [P, 16, P], F32) for i in range(2)]
        wk = sb(nc, es, "wk", [P, 256], F32)
        sv = sb(nc, es, "sv", [P, 16, 16], F32)
        si = sb(nc, es, "si", [P, 16, 16], U32)
        sif = sb(nc, es, "sif", [P, 16, 16], F32)
        cand = sb(nc, es, "cand", [P, 8, 256], F32)
        cv = sb(nc, es, "cv", [P, 8, 16], F32)
        ci = sb(nc, es, "ci", [P, 8, 16], U32)
        cu = sb(nc, es, "cu", [P, 8, 16], U32)
        ikf = sb(nc, es, "ikf", [P, 8, 16], F32)
        jkf = sb(nc, es, "jkf", [P, 8, 16], F32)
        iot_i = sb(nc, es, "iot_i", [P, 16], I32)
        iot = sb(nc, es, "iot", [P, 16], F32)
        iot3 = sb(nc, es, "iot3", [P, 16, 16], F32)
        oh = sb(nc, es, "oh", [P, 16, 16], F32)
        i1 = sb(nc, es, "i1", [P, 8, 16], F32)
        i2 = sb(nc, es, "i2", [P, 8, 16], F32)
        eidf = sb(nc, es, "eidf", [P, P], F32)
        eid = [sb(nc, es, f"eid{i}", [P, P], U32) for i in range(2)]
        gates = [sb(nc, es, f"gates{i}", [P, 8, 16], F32) for i in range(2)]
        gsum = sb(nc, es, "gsum", [P, 8], F32)
        act_ = sb(nc, es, "act_", [P, P], F32)
        g1 = sb(nc, es, "g1", [P, P], F32)
        g2 = sb(nc, es, "g2", [P, P], F32)
        coef = sb(nc, es, "coef", [P, P], F32)
        xn2 = [sb(nc, es, f"xn2{i}", [P, D], BF16) for i in range(2)]
        ht = [sb(nc, es, f"ht{i}", [P, D], F32) for i in range(2)]
        oacc = sb(nc, es, "oacc", [P, D], F32)
        junk = sb(nc, es, "junk", [P, D], F32)
        prod = [sb(nc, es, f"prod{i}", [P, D], BF16) for i in range(4)]
        ident4 = sb(nc, es, "ident4", [P, NS, P], BF16)
        dg4 = [sb(nc, es, f"dg4{i}", [P, NS, P], BF16) for i in range(3)]
        ss = sb(nc, es, "ss", [P, 2], F32)
        yt = [sb(nc, es, f"yt{i}", [P, D], F32) for i in range(2)]
        GB = [sb(nc, es, f"GB{i}", [P, NS, 2 * D], BF16) for i in range(NB)]

        kb.dma('sp', gfin[:], norm_final[0:1, :].partition_broadcast(P), r=('norm_final',), w=('gfin',))
        kb.op('dve', lambda e: e.tensor_copy(out=ident4[:], in_=ident[:, :].unsqueeze(1).to_broadcast([P, NS, P])),
              r=('ident',), w=('ident4',))
        kb.op('pool', lambda e: e.iota(iot_i[:], pattern=[[1, 16]], base=0, channel_multiplier=0), w=('iot_i',))
        kb.op('dve', lambda e: e.tensor_copy(out=iot[:], in_=iot_i[:]), r=('iot_i',), w=('iot',))
        kb.op('dve', lambda e: e.tensor_copy(out=iot3[:], in_=iot[:, :].unsqueeze(1).to_broadcast([P, 16, 16])),
              r=('iot',), w=('iot3',))

        def topk_gen(t):
            s_ = t % 2
            t0 = t * P
            kb.dma('sp', sc[s_][:], scores_d[t0:t0 + P, :].rearrange("p (a b) -> p a b", b=P), r=('scores_d',),
                   w=(('sc', s_),))
            kb.dma('sp', xn2[s_][:], xn2d[t0:t0 + P, :], r=('xn2d',), w=(('xn2', s_),))
            kb.dma('sp', ht[s_][:], hbuf[t0:t0 + P, :], r=('hbuf',), w=(('ht', s_),))
            for hc in range(16):
                src = sc[s_][:, hc, :]
                kb.op('dve', lambda e: e.max(out=sv[:, hc, 0:8], in_=src), r=(('sc', s_),), w=('sv',))
                kb.op('dve', lambda e: e.max_index(out=si[:, hc, 0:8], in_max=sv[:, hc, 0:8], in_values=src),
                      r=(('sc', s_), 'sv'), w=('si',))
                kb.op('dve', lambda e: e.match_replace(out=wk[:, 0:P], in_to_replace=sv[:, hc, 0:8], in_values=src,
                                                       imm_value=-1e30), r=(('sc', s_), 'sv'), w=('wk',))
                kb.op('dve', lambda e: e.max(out=sv[:, hc, 8:16], in_=wk[:, 0:P]), r=('wk',), w=('sv',))
                kb.op('dve', lambda e: e.max_index(out=si[:, hc, 8:16], in_max=sv[:, hc, 8:16], in_values=wk[:, 0:P]),
                      r=('wk', 'sv'), w=('si',))
                yield
            kb.op('dve', lambda e: e.tensor_copy(out=sif[:], in_=si[:]), r=('si',), w=('sif',))
            for h in range(8):
                c3 = cand[:, h, :].rearrange("p (i j) -> p i j", j=16)
                kb.op('dve', lambda e: e.tensor_copy(out=c3, in_=sv[:, 2 * h, :].unsqueeze(2).to_broadcast([P, 16, 16])),
                      r=('sv',), w=('cand',))
                kb.op('dve', lambda e: e.tensor_tensor(out=c3, in0=c3,
                                                       in1=sv[:, 2 * h + 1, :].unsqueeze(1).to_broadcast([P, 16, 16]),
                                                       op=ALU.add), r=('sv', 'cand'), w=('cand',))
                src = cand[:, h, :]
                kb.op('dve', lambda e: e.max(out=cv[:, h, 0:8], in_=src), r=('cand',), w=('cv',))
                kb.op('dve', lambda e: e.max_index(out=ci[:, h, 0:8], in_max=cv[:, h, 0:8], in_values=src),
                      r=('cand', 'cv'), w=('ci',))
                kb.op('dve', lambda e: e.match_replace(out=wk[:], in_to_replace=cv[:, h, 0:8], in_values=src,
                                                       imm_value=-1e30), r=('cand', 'cv'), w=('wk',))
                kb.op('dve', lambda e: e.max(out=cv[:, h, 8:16], in_=wk[:]), r=('wk',), w=('cv',))
                kb.op('dve', lambda e: e.max_index(out=ci[:, h, 8:16], in_max=cv[:, h, 8:16], in_values=wk[:]),
                      r=('wk', 'cv'), w=('ci',))
                yield
            kb.op('dve', lambda e: e.tensor_scalar(out=cu[:], in0=ci[:], scalar1=4, scalar2=None,
                                                   op0=ALU.logical_shift_right), r=('ci',), w=('cu',))
            kb.op('dve', lambda e: e.tensor_copy(out=ikf[:], in_=cu[:]), r=('cu',), w=('ikf',))
            kb.op('dve', lambda e: e.tensor_scalar(out=cu[:], in0=ci[:], scalar1=15, scalar2=None,
                                                   op0=ALU.bitwise_and), r=('ci',), w=('cu',))
            kb.op('dve', lambda e: e.tensor_copy(out=jkf[:], in_=cu[:]), r=('cu',), w=('jkf',))
            for h in range(8):
                for (kf, half, dst) in ((ikf, 0, i1), (jkf, 1, i2)):
                    kb.op('dve', lambda e: e.tensor_tensor(out=oh[:], in0=iot3[:],
                                                           in1=kf[:, h, :].unsqueeze(2).to_broadcast([P, 16, 16]),
                                                           op=ALU.is_equal), r=('iot3', 'ikf', 'jkf'), w=('oh',))
                    kb.op('dve', lambda e: e.tensor_tensor(out=oh[:], in0=oh[:],
                                                           in1=sif[:, 2 * h + half, :].unsqueeze(1).to_broadcast([P, 16, 16]),
                                                           op=ALU.mult), r=('oh', 'sif'), w=('oh',))
                    kb.op('dve', lambda e: e.tensor_reduce(out=dst[:, h, :], in_=oh[:], axis=AX.X, op=ALU.add),
                          r=('oh',), w=('i1', 'i2'))
                yield
            kb.op('dve', lambda e: e.scalar_tensor_tensor(out=eidf[:], in0=i1[:].rearrange("p a b -> p (a b)"),
                                                          scalar=128.0, in1=i2[:].rearrange("p a b -> p (a b)"),
                                                          op0=ALU.mult, op1=ALU.add), r=('i1', 'i2'), w=('eidf',))
            kb.op('dve', lambda e: e.tensor_copy(out=eid[s_][:], in_=eidf[:]), r=('eidf',), w=(('eid', s_),))
            gt = gates[s_]
            gk = ('gates', s_)
            kb.op('dve', lambda e: e.tensor_tensor(out=gt[:], in0=cv[:], in1=cv[:, :, 0:1].to_broadcast([P, 8, 16]),
                                                   op=ALU.subtract), r=('cv',), w=(gk,))
            kb.op('act', lambda e: e.activation(out=gt[:], in_=gt[:], func=AF.Exp), r=(gk,), w=(gk,))
            kb.op('dve', lambda e: e.tensor_reduce(out=gsum[:], in_=gt[:], axis=AX.X, op=ALU.add), r=(gk,),
                  w=('gsum',))
            kb.op('dve', lambda e: e.reciprocal(out=gsum[:], in_=gsum[:]), r=('gsum',), w=('gsum',))
            kb.op('dve', lambda e: e.tensor_tensor(out=gt[:], in0=gt[:],
                                                   in1=gsum[:, :].unsqueeze(2).to_broadcast([P, 8, 16]), op=ALU.mult),
                  r=(gk, 'gsum'), w=(gk,))

        def topk(t):
            for _ in topk_gen(t):
                pass

        gi = [0]

        def gather(e_, s0):
            b = gi[0] % NB
            gi[0] += 1
            for q in range(NS):
                kb.dma('pool', GB[b][:, q, :], uvb_d[:, :], r=(('eid', e_),), w=(('GB', b),), acc=(q > 0),
                       indirect=bass.IndirectOffsetOnAxis(ap=eid[e_][:, s0 + q:s0 + q + 1], axis=0))
            return b

        nb_t = P // NS
        di = [0]

        def stage_a(t, k, idx):
            s_ = t % 2
            s0 = k * NS
            b = gather(s_, s0)
            kk = idx % 2
            a4 = act4[kk]
            for q in range(NS):
                pq = q % 4
                kb.op('dve', lambda e: e.tensor_tensor(out=prod[pq][:], in0=GB[b][:, q, 0:D], in1=xn2[s_][:],
                                                       op=ALU.mult), r=(('GB', b), ('xn2', s_)), w=(('prod', pq),))
                kb.op('act', lambda e: e.activation(out=junk[:], in_=prod[pq][:], func=AF.Copy,
                                                    accum_out=a4[:, q:q + 1]),
                      r=(('prod', pq),), w=(('a4', kk, q),))
            return b

        def stage_b(t, k, idx, b):
            s_ = t % 2
            s0 = k * NS
            kk = idx % 2
            a4, g1, g2, c4 = act4[kk], g14[kk], g24[kk], c44[kk]
            ak = tuple(('a4', kk, q) for q in range(NS))
            kb.op('dve', lambda e: e.tensor_tensor(out=g1[:], in0=a4[:], in1=a4[:], op=ALU.mult), r=ak,
                  w=(('g1', kk),))
            kb.op('dve', lambda e: e.tensor_scalar(out=g1[:], in0=g1[:], scalar1=0.044715, scalar2=1.0,
                                                   op0=ALU.mult, op1=ALU.add), r=(('g1', kk),), w=(('g1', kk),))
            kb.op('dve', lambda e: e.tensor_tensor(out=g1[:], in0=g1[:], in1=a4[:], op=ALU.mult),
                  r=(('g1', kk),) + ak, w=(('g1', kk),))
            kb.op('act', lambda e: e.activation(out=g2[:], in_=g1[:], func=AF.Tanh, scale=GC), r=(('g1', kk),),
                  w=(('g2', kk),))
            kb.op('dve', lambda e: e.scalar_tensor_tensor(out=g2[:], in0=g2[:], scalar=1.0, in1=a4[:],
                                                          op0=ALU.add, op1=ALU.mult),
                  r=(('g2', kk),) + ak, w=(('g2', kk),))
            gflat = gates[s_][:].rearrange("p a b -> p (a b)")
            kb.op('dve', lambda e: e.scalar_tensor_tensor(out=c4[:], in0=g2[:], scalar=0.5,
                                                          in1=gflat[:, s0:s0 + NS], op0=ALU.mult, op1=ALU.mult),
                  r=(('g2', kk), ('gates', s_)), w=(('c4', kk),))
            r_ = di[0] % 3
            di[0] += 1
            kb.op('dve', lambda e: e.tensor_tensor(out=dg4[r_][:], in0=ident4[:],
                                                   in1=c4[:, :].unsqueeze(2).to_broadcast([P, NS, P]), op=ALU.mult),
                  r=('ident4', ('c4', kk)), w=(('dg4', r_),))
            for q in range(NS):
                slot = s0 + q
                for nh in range(2):
                    kb.op('pe', lambda e: e.matmul(pO[s_][nh][:], lhsT=dg4[r_][:, q, :],
                                                   rhs=GB[b][:, q, D + nh * 512:D + (nh + 1) * 512],
                                                   start=(slot == 0), stop=(slot == P - 1)),
                          r=(('dg4', r_), ('GB', b)), w=(('pO', s_, nh),))

        def tile_end(t):
            s_ = t % 2
            t0 = t * P
            for nh in range(2):
                kb.op('dve', lambda e: e.tensor_tensor(out=oacc[:, nh * 512:(nh + 1) * 512], in0=pO[s_][nh][:],
                                                       in1=ht[s_][:, nh * 512:(nh + 1) * 512], op=ALU.add),
                      r=(('pO', s_, nh), ('ht', s_)), w=('oacc',))
            kb.op('act', lambda e: e.activation(out=junk[:], in_=oacc[:], func=AF.Square, scale=D ** -0.5,
                                                accum_out=ss[:, 0:1]), r=('oacc',), w=('junk', 'ss'))
            kb.op('dve', lambda e: e.tensor_scalar(out=ss[:, 1:2], in0=ss[:, 0:1], scalar1=EPS, scalar2=None,
                                                   op0=ALU.add), r=('ss',), w=('ss',))
            kb.op('act', lambda e: e.activation(out=ss[:, 1:2], in_=ss[:, 1:2], func=AF.Sqrt), r=('ss',), w=('ss',))
            kb.op('dve', lambda e: e.reciprocal(out=ss[:, 1:2], in_=ss[:, 1:2]), r=('ss',), w=('ss',))
            kb.op('dve', lambda e: e.scalar_tensor_tensor(out=yt[s_][:], in0=oacc[:], scalar=ss[:, 1:2], in1=gfin[:],
                                                          op0=ALU.mult, op1=ALU.mult), r=('oacc', 'ss', 'gfin'),
                  w=(('yt', s_),))
            kb.dma('sp', y[t0:t0 + P, :], yt[s_][:], r=(('yt', s_),), w=('y',), acc=True)

        batches = [(t, k) for t in range(nt) for k in range(nb_t)]
        topk(0)
        tkg = None
        cur_b = stage_a(batches[0][0], batches[0][1], 0)
        for idx, (t, k) in enumerate(batches):
            nxt_b = None
            if idx + 1 < len(batches):
                nxt_b = stage_a(batches[idx + 1][0], batches[idx + 1][1], idx + 1)
            stage_b(t, k, idx, cur_b)
            if k == 2 and t + 1 < nt:
                tkg = topk_gen(t + 1)
            if tkg is not None and k >= 2:
                for _ in range(2):
                    try:
                        next(tkg)
                    except StopIteration:
                        tkg = None
                        break
            if k == nb_t - 2 and tkg is not None:
                for _ in tkg:
                    pass
                tkg = None
            if k == nb_t - 1:
                tile_end(t)
            cur_b = nxt_b


def build(S, dbg=False, phases="pabcde"):
    nc = bass.Bass("TRN2", target_bir_lowering=False)
    okind = "ExternalOutput" if dbg else "Internal"
    nch = S // P

    def inp(name, shape, dt=F32):
        return nc.dram_tensor(name, list(shape), dt, kind="ExternalInput").ap()

    def scr(name, shape, dt, out=False):
        return nc.dram_tensor(name, list(shape), dt, kind=okind if not out else "ExternalOutput").ap()

    x = inp("x", [S, D])
    norm_mix = inp("norm_mix", [1, D])
    w_in = inp("w_in", [D, NW])
    rel_bias = inp("rel_bias", [32, 12])
    c_ohv = inp("c_ohv", [33, 6, 256])
    dn_conv = inp("dn_conv", [4, 1536])
    dn_a_log = inp("dn_a_log", [2, 4])
    dn_dt_bias = inp("dn_dt_bias", [2, 4])
    dn_out_norm = inp("dn_out_norm", [1, P])
    w_bdn = inp("w_branch_dn", [512, D])
    w_bda = inp("w_branch_da", [512, D])
    w_out = inp("w_out", [D, D])
    norm_ffn = inp("norm_ffn", [1, D])
    peer_wq = inp("peer_wq", [D, 2048])
    sub_keys = inp("peer_sub_keys", [16, P, P])
    peer_u = inp("peer_u", [16384, D])
    peer_v = inp("peer_v", [16384, D])
    norm_final = inp("norm_final", [1, D])
    projT = scr("projT", [68, P, S], BF16)
    abT = scr("abT", [16, S], F32)
    hv = scr("hv", [24, 256], F32)
    odaT = scr("odaT", [4, P, S], BF16)
    gam = scr("gam", [2, 4, S], F32)
    tokS_d = scr("tokS_d", [P, nch, 40], F32)
    glb_d = scr("glb_d", [P, 8, nch], F32)
    dnq = scr("dnq", [4, P, S], BF16)
    dnk = scr("dnk", [4, P, S], BF16)
    dnkt = scr("dnkt", [4, P, nch, P], BF16)
    dnvt = scr("dnvt", [4, P, nch, P], BF16)
    odnT = scr("odnT", [4, P, S], BF16)
    hbuf = scr("hbuf", [S, D], F32)
    xn2d = scr("xn2d", [S, D], BF16)
    scores_d = scr("scores_d", [S, 2048], F32)
    y = scr("y", [S, D], F32, out=True)
    uvb_d = scr("uvb_d", [16384, 2 * D], BF16)
    with ExitStack() as es:
        kb = KB(nc, es)
        phase_a(nc, kb, S, x, w_in, norm_mix, projT, abT)
        kb.barrier()
        if "b" in phases:
            phase_b0(nc, kb, S, abT, dn_a_log, dn_dt_bias, gam, tokS_d, glb_d)
            kb.barrier()
            with ExitStack() as es_p:
                pg = None
                if "p" in phases:
                    p_tiles = ([sb(nc, es_p, f"pst{i}", [P, 2, D], F32) for i in range(4)],
                               [sb(nc, es_p, f"pob{i}", [P, 2, D], BF16) for i in range(4)])
                    pg = phase_p_gen(nc, kb, p_tiles, peer_u, peer_v, uvb_d)
                phase_b1(nc, kb, S, projT, dn_conv, dnq, dnk, dnkt, dnvt, bg=pg)
                if pg is not None:
                    for _ in pg:
                        pass
                kb.barrier()
            phase_b2(nc, kb, S, projT, dnq, dnk, dnkt, dnvt, gam, tokS_d, glb_d, dn_out_norm, odnT)
            kb.barrier()
        if "c" in phases:
            phase_c(nc, kb, S, projT, rel_bias, c_ohv, hv, odaT)
            kb.barrier()
        if "d" in phases:
            phase_d(nc, kb, S, x, projT, odnT, odaT, w_bdn, w_bda, w_out, norm_ffn, peer_wq, sub_keys, hbuf, xn2d,
                    scores_d)
            kb.barrier()
        if "e" in phases:
            phase_e(nc, kb, S, hbuf, xn2d, scores_d, uvb_d, norm_final, y)
            kb.barrier()
        print("ninst", kb.ninst, "nsem", kb.nsem)
    return nc


def make_in_map(inputs, b, S):
    f = lambda a: np.ascontiguousarray(np.asarray(a, dtype=np.float32))
    return {
        "x": f(inputs["x"][b, :S]),
        "norm_mix": f(inputs["norm_mix"]).reshape(1, D),
        "w_in": f(inputs["w_in"]).reshape(D, NW),
        "rel_bias": f(inputs["rel_bias"]),
        "c_ohv": make_ohv(),
        "dn_conv": f(inputs["dn_conv"]).reshape(4, 1536),
        "dn_a_log": f(inputs["dn_a_log"]).reshape(2, 4),
        "dn_dt_bias": f(inputs["dn_dt_bias"]).reshape(2, 4),
        "dn_out_norm": f(inputs["dn_out_norm"]).reshape(1, P),
        "w_branch_dn": f(inputs["w_branch_dn"]).reshape(512, D),
        "w_branch_da": f(inputs["w_branch_da"]).reshape(512, D),
        "w_out": f(inputs["w_out"]).reshape(D, D),
        "norm_ffn": f(inputs["norm_ffn"]).reshape(1, D),
        "peer_wq": f(inputs["peer_wq"]).reshape(D, 2048),
        "peer_sub_keys": f(inputs["peer_sub_keys"]).reshape(16, P, P),
        "peer_u": f(inputs["peer_u"]).reshape(16384, D),
        "peer_v": f(inputs["peer_v"]).reshape(16384, D),
        "norm_final": f(inputs["norm_final"]).reshape(1, D),
    }


def kernel(**inputs):
    B, S = inputs["x"].shape[0], inputs["x"].shape[1]
    nc = build(S)
    base = make_in_map(inputs, 0, S)
    in_maps = []
    for b in range(B):
        m = dict(base)
        m["x"] = np.ascontiguousarray(np.asarray(inputs["x"][b], dtype=np.float32))
        in_maps.append(m)
    res = run_bass_kernel_spmd(nc, in_maps, core_ids=list(range(B)))
    return np.stack([np.asarray(r["y"], dtype=np.float32) for r in res.results], axis=0)
```
